# Optimizing a Trainium2 kernel written in Bass

```python
import jax, jax.numpy as jnp
from jax import lax
import numpy as np

D_MODEL = 4096
BATCH = 2
SEQ = 4096
DEPTH = 1

GRID_W = 64
CTX_LEN = 256
NA_HEADS = 16
NA_HEAD_DIM = 128
NA_WIN_R = 8
NA_WIN_C = 16
RET_HEADS = 8
RET_QK_DIM = 256
RET_V_DIM = 256
RET_CHUNK = 128
RET_DECAY_BASE_EXP = 5.0
N_EXPERTS = 16
EXPERT_FF = 2048
EC_CAPACITY_FACTOR = 2
ROPE_BASE = 10000.0
NORM_EPS = 1e-6
NEG_INF = -1e30
N_MOD = 6

NA_WIDTH = NA_HEADS * NA_HEAD_DIM
RET_QK_WIDTH = RET_HEADS * RET_QK_DIM
RET_V_WIDTH = RET_HEADS * RET_V_DIM
NA_Q0 = 0
NA_K0 = NA_Q0 + NA_WIDTH
NA_V0 = NA_K0 + NA_WIDTH
RET_Q0 = NA_V0 + NA_WIDTH
RET_K0 = RET_Q0 + RET_QK_WIDTH
RET_V0 = RET_K0 + RET_QK_WIDTH
RET_GF0 = RET_V0 + RET_V_WIDTH
RET_GB0 = RET_GF0 + RET_V_WIDTH
GATE_A0 = RET_GB0 + RET_V_WIDTH
GATE_B0 = GATE_A0 + D_MODEL
D_IN = GATE_B0 + D_MODEL

kernel_name = "hybrid_na_retention_ec_moe_block"


def rmsnorm(x, g):
    xf = x.astype(jnp.float32)
    y = xf * lax.rsqrt(jnp.mean(xf * xf, axis=-1, keepdims=True) + NORM_EPS)
    return (y * g.astype(jnp.float32)).astype(x.dtype)


def head_rmsnorm(o):
    of = o.astype(jnp.float32)
    return of * lax.rsqrt(jnp.mean(of * of, axis=-1, keepdims=True) + NORM_EPS)


def modulate(h, shift, scale):
    return h * (1 + scale) + shift


def heads(a, n):
    b, t, _ = a.shape
    return a.reshape(b, t, n, -1).transpose(0, 2, 1, 3)


def merge_heads(a):
    b, h, t, d = a.shape
    return a.transpose(0, 2, 1, 3).reshape(b, t, h * d)


def split_in(proj):
    bounds = (NA_K0, NA_V0, RET_Q0, RET_K0, RET_V0, RET_GF0, RET_GB0, GATE_A0, GATE_B0)
    return jnp.split(proj, bounds, axis=-1)


def rope_1d(a, pos):
    half = a.shape[-1] // 2
    inv = ROPE_BASE ** (-jnp.arange(half, dtype=jnp.float32) / half)
    ang = pos[:, None] * inv[None, :]
    cos, sin = jnp.cos(ang), jnp.sin(ang)
    a1, a2 = a[..., :half], a[..., half:]
    return jnp.concatenate([a1 * cos - a2 * sin, a1 * sin + a2 * cos], axis=-1).astype(a.dtype)


def axial_rope(a, row_pos, col_pos):
    half = a.shape[-1] // 2
    return jnp.concatenate([rope_1d(a[..., :half], row_pos), rope_1d(a[..., half:], col_pos)], axis=-1)


def neighbourhood_attention(q, k, v, k_ctx, v_ctx, rpb):
    b, h, t, dh = q.shape
    rows = t // GRID_W
    wr = min(NA_WIN_R, rows)
    scale = dh ** -0.5
    qg = q.reshape(b, h, rows, GRID_W, dh)
    kg = k.reshape(b, h, rows, GRID_W, dh)
    vg = v.reshape(b, h, rows, GRID_W, dh)
    r = jnp.arange(rows)
    r0 = jnp.clip(r - wr // 2, 0, rows - wr)
    key_rows = r0[:, None] + jnp.arange(wr)[None, :]
    k_win = kg[:, :, key_rows]
    v_win = vg[:, :, key_rows]
    cidx = jnp.arange(GRID_W)
    c0 = jnp.clip(cidx - NA_WIN_C // 2, 0, GRID_W - NA_WIN_C)
    col_ok = (cidx[None, :] >= c0[:, None]) & (cidx[None, :] < c0[:, None] + NA_WIN_C)
    dr = key_rows - r[:, None] + (NA_WIN_R - 1)
    dc = jnp.clip(cidx[None, :] - cidx[:, None], -(NA_WIN_C - 1), NA_WIN_C - 1) + (NA_WIN_C - 1)
    bias = rpb[:, dr[:, None, :, None], dc[None, :, None, :]]
    s_win = jnp.einsum('bhrqd,bhrwkd->bhrqwk', qg, k_win).astype(jnp.float32) * scale + bias[None].astype(jnp.float32)
    s_win = jnp.where(col_ok[:, None, :], s_win, NEG_INF)
    n_win = wr * GRID_W
    s_win = s_win.reshape(b, h, rows, GRID_W, n_win)
    s_ctx = jnp.einsum('bhrqd,bhcd->bhrqc', qg, k_ctx).astype(jnp.float32) * scale
    p = jax.nn.softmax(jnp.concatenate([s_win, s_ctx], axis=-1), axis=-1)
    p_win = p[..., :n_win].reshape(b, h, rows, GRID_W, wr, GRID_W).astype(v.dtype)
    p_ctx = p[..., n_win:].astype(v.dtype)
    out = jnp.einsum('bhrqwk,bhrwkd->bhrqd', p_win, v_win) + jnp.einsum('bhrqc,bhcd->bhrqd', p_ctx, v_ctx)
    return out.reshape(b, h, t, dh)


def context_attention(q, k, v):
    s = jnp.einsum('bhqd,bhkd->bhqk', q, k).astype(jnp.float32) * (q.shape[-1] ** -0.5)
    p = jax.nn.softmax(s, axis=-1).astype(v.dtype)
    return jnp.einsum('bhqk,bhkd->bhqd', p, v)


def retention_log_decay(decay_exp):
    return jnp.log1p(-jnp.exp2(-decay_exp.astype(jnp.float32)))


def context_final_state(k, v, log_gamma, reverse):
    tc = k.shape[2]
    t = jnp.arange(tc, dtype=jnp.float32)
    steps = t if reverse else (tc - 1.0 - t)
    w = jnp.exp(log_gamma[:, None] * steps[None, :])
    return jnp.einsum('bhtk,bhtv,ht->bhkv', k.astype(jnp.float32), v.astype(jnp.float32), w)


def chunk_retention(q, k, v, log_gamma, s0):
    b, h, t, _ = q.shape
    dv = v.shape[-1]
    n = t // RET_CHUNK
    pos = jnp.arange(RET_CHUNK, dtype=jnp.float32)
    diff = pos[:, None] - pos[None, :]
    decay_in = jnp.where(diff >= 0, jnp.exp(log_gamma[:, None, None] * jnp.maximum(diff, 0.0)), 0.0)
    q_dec = jnp.exp(log_gamma[:, None] * (pos + 1.0))[..., None]
    k_dec = jnp.exp(log_gamma[:, None] * (RET_CHUNK - 1.0 - pos))[..., None]
    chunk_dec = jnp.exp(log_gamma * RET_CHUNK)[:, None, None]

    def to_chunks(a):
        return jnp.moveaxis(a.reshape(b, h, n, RET_CHUNK, a.shape[-1]), 2, 0)

    def step(s, inp):
        qb, kb, vb = inp
        scores = jnp.einsum('bhid,bhjd->bhij', qb, kb) * decay_in
        o = jnp.einsum('bhij,bhjv->bhiv', scores, vb) + jnp.einsum('bhid,bhdv->bhiv', qb, s) * q_dec
        s = s * chunk_dec + jnp.einsum('bhjd,bhjv->bhdv', kb * k_dec, vb)
        return s, o

    _, out = lax.scan(step, s0.astype(jnp.float32), (to_chunks(q), to_chunks(k), to_chunks(v)))
    return jnp.moveaxis(out, 0, 2).reshape(b, h, t, dv)


def bidirectional_retention(q, k, v, log_gamma, s_f, s_b):
    rev = lambda a: jnp.flip(a, axis=2)
    o_f = chunk_retention(q, k, v, log_gamma[0], s_f)
    o_b = rev(chunk_retention(rev(q), rev(k), rev(v), log_gamma[1], s_b))
    return o_f, o_b


def merge_branches(y_na, o_f, o_b, g_rf, g_rb, gate_a, gate_b, w_branch_na, w_branch_ret, w_out):
    y_ret = (jax.nn.silu(g_rf) * merge_heads(head_rmsnorm(o_f))
             + jax.nn.silu(g_rb) * merge_heads(head_rmsnorm(o_b))).astype(y_na.dtype)
    mixed = jax.nn.sigmoid(gate_a) * (y_na @ w_branch_na) + jax.nn.sigmoid(gate_b) * (y_ret @ w_branch_ret)
    return mixed @ w_out


def expert_choice_ffn(h, w_router, w_gate, w_up, w_down):
    b, t, d = h.shape
    cap = EC_CAPACITY_FACTOR * t // N_EXPERTS
    aff = jax.nn.softmax((h @ w_router).astype(jnp.float32), axis=-1)
    g, idx = lax.top_k(aff.transpose(0, 2, 1), cap)
    xe = jax.vmap(lambda hb, ib: hb[ib])(h, idx)
    a = jnp.einsum('becd,edf->becf', xe, w_gate)
    u = jnp.einsum('becd,edf->becf', xe, w_up)
    ye = jnp.einsum('becf,efd->becd', jax.nn.silu(a) * u, w_down) * g[..., None].astype(h.dtype)
    return jax.vmap(lambda yb, ib: jnp.zeros((t, d), yb.dtype).at[ib.reshape(-1)].add(yb.reshape(-1, d)))(ye, idx)


def hybrid_layer(x, cx, c, c_ctx, norm1, norm2, w_mod, b_mod, w_in, na_rpb, ret_decay,
                 w_branch_na, w_branch_ret, w_out, w_router, w_gate, w_up, w_down, need_ctx_out):
    b, t, _ = x.shape
    mod = jax.nn.silu(c) @ w_mod + b_mod
    mod_c = jax.nn.silu(c_ctx) @ w_mod + b_mod
    sh1, sc1, g1, sh2, sc2, g2 = [m[:, None, :] for m in jnp.split(mod, N_MOD, axis=-1)]
    csh1, csc1, cg1, csh2, csc2, cg2 = jnp.split(mod_c, N_MOD, axis=-1)

    h = modulate(rmsnorm(x, norm1), sh1, sc1)
    hc = modulate(rmsnorm(cx, norm1), csh1, csc1)
    q_a, k_a, v_a, q_r, k_r, v_r, g_rf, g_rb, gate_a, gate_b = split_in(h @ w_in)
    if need_ctx_out:
        q_ac, k_ac, v_ac, q_rc, k_rc, v_rc, g_rfc, g_rbc, gate_ac, gate_bc = split_in(hc @ w_in)
    else:
        k_ac, v_ac = jnp.split(hc @ w_in[:, NA_K0:RET_Q0], 2, axis=-1)
        k_rc, v_rc = jnp.split(hc @ w_in[:, RET_K0:RET_GF0], (RET_QK_WIDTH,), axis=-1)

    ka_c, va_c = heads(k_ac, NA_HEADS), heads(v_ac, NA_HEADS)
    y_na = merge_heads(neighbourhood_attention(heads(q_a, NA_HEADS), heads(k_a, NA_HEADS), heads(v_a, NA_HEADS),
                                               ka_c, va_c, na_rpb))

    log_gamma = retention_log_decay(ret_decay)
    tpos = jnp.arange(t)
    row_pos = (tpos // GRID_W).astype(jnp.float32)
    col_pos = (tpos % GRID_W).astype(jnp.float32)
    k_scale = RET_QK_DIM ** -0.5
    qr = axial_rope(heads(q_r, RET_HEADS), row_pos, col_pos)
    kr = axial_rope(heads(k_r, RET_HEADS), row_pos, col_pos) * k_scale
    vr = heads(v_r, RET_HEADS)
    krc = heads(k_rc, RET_HEADS) * k_scale
    vrc = heads(v_rc, RET_HEADS)
    s_f = context_final_state(krc, vrc, log_gamma[0], reverse=False)
    s_b = context_final_state(krc, vrc, log_gamma[1], reverse=True)
    o_f, o_b = bidirectional_retention(qr, kr, vr, log_gamma, s_f, s_b)

    x = x + g1 * merge_branches(y_na, o_f, o_b, g_rf, g_rb, gate_a, gate_b, w_branch_na, w_branch_ret, w_out)

    h2 = modulate(rmsnorm(x, norm2), sh2, sc2)
    x = x + g2 * expert_choice_ffn(h2, w_router, w_gate, w_up, w_down)

    if need_ctx_out:
        yc_na = merge_heads(context_attention(heads(q_ac, NA_HEADS), ka_c, va_c))
        zero_state = jnp.zeros_like(s_f)
        oc_f, oc_b = bidirectional_retention(heads(q_rc, RET_HEADS), krc, vrc, log_gamma, zero_state, zero_state)
        cx = cx + cg1 * merge_branches(yc_na, oc_f, oc_b, g_rfc, g_rbc, gate_ac, gate_bc,
                                       w_branch_na, w_branch_ret, w_out)
        hc2 = modulate(rmsnorm(cx, norm2), csh2, csc2)
        cx = cx + cg2 * expert_choice_ffn(hc2, w_router, w_gate, w_up, w_down)
    return x, cx


def setup_inputs(seed: int = 0) -> dict:
    key = jax.random.key(seed)
    ks = jax.random.split(key, 20)
    f32 = jnp.float32

    def nrm(k, shape, scale):
        return jax.random.normal(k, shape, f32) * scale

    D = D_MODEL
    return {
        "x": nrm(ks[0], (BATCH, SEQ, D), 1.0),
        "c": nrm(ks[1], (BATCH, D), 1.0),
        "ctx": nrm(ks[2], (BATCH, CTX_LEN, D), 1.0),
        "c_ctx": nrm(ks[3], (D,), 1.0),
        "norm1": 1.0 + nrm(ks[4], (DEPTH, D), 0.05),
        "norm2": 1.0 + nrm(ks[5], (DEPTH, D), 0.05),
        "w_mod": nrm(ks[6], (DEPTH, D, N_MOD * D), 0.5 * D ** -0.5),
        "b_mod": nrm(ks[7], (DEPTH, N_MOD * D), 0.02),
        "w_in": nrm(ks[8], (DEPTH, D, D_IN), D ** -0.5),
        "na_rpb": nrm(ks[9], (DEPTH, NA_HEADS, 2 * NA_WIN_R - 1, 2 * NA_WIN_C - 1), 0.1),
        "ret_decay": RET_DECAY_BASE_EXP + jnp.arange(RET_HEADS, dtype=f32)[None, None, :]
                     + nrm(ks[10], (DEPTH, 2, RET_HEADS), 0.1),
        "w_branch_na": nrm(ks[11], (DEPTH, NA_WIDTH, D), NA_WIDTH ** -0.5),
        "w_branch_ret": nrm(ks[12], (DEPTH, RET_V_WIDTH, D), RET_V_WIDTH ** -0.5),
        "w_out": nrm(ks[13], (DEPTH, D, D), D ** -0.5),
        "w_router": nrm(ks[14], (DEPTH, D, N_EXPERTS), D ** -0.5),
        "w_gate": nrm(ks[15], (DEPTH, N_EXPERTS, D, EXPERT_FF), D ** -0.5),
        "w_up": nrm(ks[16], (DEPTH, N_EXPERTS, D, EXPERT_FF), D ** -0.5),
        "w_down": nrm(ks[17], (DEPTH, N_EXPERTS, EXPERT_FF, D), EXPERT_FF ** -0.5),
        "final_norm": 1.0 + nrm(ks[18], (D,), 0.05),
    }


def reference(x, c, ctx, c_ctx, norm1, norm2, w_mod, b_mod, w_in, na_rpb, ret_decay,
              w_branch_na, w_branch_ret, w_out, w_router, w_gate, w_up, w_down, final_norm):
    for layer in range(DEPTH):
        x, ctx = hybrid_layer(x, ctx, c, c_ctx, norm1[layer], norm2[layer], w_mod[layer], b_mod[layer],
                              w_in[layer], na_rpb[layer], ret_decay[layer], w_branch_na[layer],
                              w_branch_ret[layer], w_out[layer], w_router[layer], w_gate[layer],
                              w_up[layer], w_down[layer], need_ctx_out=layer + 1 < DEPTH)
    return rmsnorm(x, final_norm)
```

```python
import contextlib
import numpy as np
import concourse.bass as bass
import concourse.mybir as mybir
from concourse.bass_utils import run_bass_kernel_spmd

F32 = mybir.dt.float32
BF16 = mybir.dt.bfloat16
I32 = mybir.dt.int32
AF = mybir.ActivationFunctionType
ALU = mybir.AluOpType
AX = mybir.AxisListType

D = 4096
KC = 32
NTOK = 1024
NKV = 1536
NCTX = 256
EPS = 1e-6
DIN = 24576
DBG = ()
PHASES = ("P1", "P2", "NA", "R", "G", "N2", "E", "C")
XMID_IN = False
SKIP_IN = ()


class Res:
    __slots__ = ("name", "w", "r", "multi", "ws")

    def __init__(self, name="", multi=False):
        self.name = name
        self.w = None
        self.r = []
        self.multi = multi
        self.ws = {}


class FW:
    EPOCH = 3000

    def __init__(self, nc, stack, n_dma_sems=64):
        self.nc = nc
        self.stack = stack
        self.E = {"pe": nc.tensor, "act": nc.scalar, "dve": nc.vector, "pool": nc.gpsimd, "sp": nc.sync}
        self.sem = {}
        self.cnt = {}
        self.ep = {}
        self.final = {}
        for e in ("pe", "act", "dve", "pool"):
            self.ep[e] = 0
            self.sem[(e, 0)] = stack.enter_context(nc.semaphore("s_%s_0" % e))
            self.cnt[e] = 0
        self.dsem = [stack.enter_context(nc.semaphore("d%d" % i)) for i in range(n_dma_sems)]
        self.dcnt = [0] * n_dma_sems
        self.dnext = 0
        self.seen = {e: {} for e in self.E}
        self.log = {e: [] for e in self.E}

    def _wait(self, eng, tok):
        if tok is None:
            return
        kind, key, n = tok
        if kind == "e":
            if key[0] == eng and eng == "pe":
                return
            sem = self.sem[key]
        else:
            sem = self.dsem[key]
        k = (kind, key)
        if self.seen[eng].get(k, 0) >= n:
            return
        self.E[eng].wait_ge(sem, n)
        self.log[eng].append(("w", k, n))
        self.seen[eng][k] = n

    def deps(self, eng, reads, writes):
        for r in reads:
            if r.multi:
                for k, n in r.ws.items():
                    self._wait(eng, (k[0], k[1], n))
            else:
                self._wait(eng, r.w)
        for w in writes:
            if not w.multi:
                self._wait(eng, w.w)
            for t in w.r:
                self._wait(eng, t)

    def _mark(self, tok, reads, writes):
        for r in reads:
            r.r.append(tok)
            if len(r.r) > 64:
                r.r = r.r[-48:]
        for w in writes:
            if w.multi:
                k = (tok[0], tok[1])
                if w.ws.get(k, 0) < tok[2]:
                    w.ws[k] = tok[2]
                if w.r:
                    w.r = []
            else:
                w.w = tok
                w.r = []

    def op(self, eng, ins_fn, reads=(), writes=(), signal=True):
        self.deps(eng, reads, writes)
        ins = ins_fn()
        if signal:
            self.cnt[eng] += 1
            ins.then_inc(self.sem[(eng, self.ep[eng])], 1)
            tok = ("e", (eng, self.ep[eng]), self.cnt[eng])
            self.log[eng].append(("i", ("e", (eng, self.ep[eng])), 1))
            if self.cnt[eng] >= self.EPOCH:
                self.ep[eng] += 1
                self.cnt[eng] = 0
                self.sem[(eng, self.ep[eng])] = self.stack.enter_context(self.nc.semaphore("s_%s_%d" % (eng, self.ep[eng])))
        else:
            tok = ("e", (eng, self.ep[eng]), self.cnt[eng] + 1)
        self._mark(tok, reads, writes)
        return ins

    def _dpre(self, q):
        i = self.dnext
        if self.dcnt[i] > 0:
            self._wait(q, ("d", i, self.dcnt[i]))

    def _dtok(self, ins, inc):
        i = self.dnext
        self.dnext = (self.dnext + 1) % len(self.dsem)
        self.dcnt[i] += inc
        ins.then_inc(self.dsem[i], inc)
        self.log[self._curq].append(("i", ("d", i), inc))
        return ("d", i, self.dcnt[i])

    def dma(self, q, out, in_, reads=(), writes=(), **kw):
        self._curq = q
        self.deps(q, reads, writes)
        self._dpre(q)
        ins = self.E[q].dma_start(out=out, in_=in_, **kw)
        tok = self._dtok(ins, 16)
        self._mark(tok, reads, writes)
        return tok

    def custom(self, q, fn, inc, reads=(), writes=()):
        self._curq = q
        self.deps(q, reads, writes)
        self._dpre(q)
        ins = fn()
        tok = self._dtok(ins, inc)
        self._mark(tok, reads, writes)
        return tok

    def barrier(self):
        for e in self.E:
            for e2 in self.cnt:
                if self.cnt[e2] > 0:
                    self._wait(e, ("e", (e2, self.ep[e2]), self.cnt[e2]))
                elif self.ep[e2] > 0:
                    self._wait(e, ("e", (e2, self.ep[e2] - 1), self.EPOCH))
            for i, c in enumerate(self.dcnt):
                if c > 0:
                    self._wait(e, ("d", i, c))


class Rot:
    def __init__(self, tiles):
        self.t = tiles
        self.r = [Res() for _ in tiles]
        self.i = 0

    def next(self):
        k = self.i % len(self.t)
        self.i += 1
        return self.t[k], self.r[k]


def build_nc():
    nc = bass.Bass("TRN2", target_bir_lowering=False)

    def din(name, shape, dt=F32):
        if name in SKIP_IN:
            return None
        return nc.dram_tensor(name, list(shape), dt, kind="ExternalInput").ap()

    def dscr(name, shape, dt):
        kind = "ExternalOutput" if name in DBG else "Internal"
        return nc.dram_tensor(name, list(shape), dt, kind=kind).ap()

    x_own = din("x_own", [NTOK, D]); x_kv = din("x_kv", [NKV, D]); ctxb = din("ctxb", [NCTX, D])
    cT = din("cT", [128, KC, 2])
    w_mod_s = din("w_mod_s", [D, 6144]); bmod_row = din("bmod_row", [1, 6144])
    norm1_row = din("norm1_row", [1, D]); norm2_row = din("norm2_row", [1, D]); fnorm_row = din("fnorm_row", [1, D])
    w_in = din("w_in", [D, DIN])
    ropeC = din("ropeC", [NTOK, 128]); ropeS = din("ropeS", [NTOK, 128])
    ropeCk = din("ropeCk", [NTOK, 128]); ropeSk = din("ropeSk", [NTOK, 128])
    nabias = din("nabias", [16, 128, 7 * 128])
    validB = din("validB", [128, NTOK]); onehotA = din("onehotA", [128, NKV])
    ret_decay = din("ret_decay", [1, 16])
    retM = din("retM", [128, 256])
    retpos = din("retpos", [128, 26])
    ccE = din("ccE", [1, 10]); ccM = din("ccM", [1, 10])
    w_bna = din("w_bna", [2048, D]); w_bret = din("w_bret", [2048, D]); w_out = din("w_out", [D, D])
    w_router = din("w_router", [D, 16])
    w_gate_s = din("w_gate_s", [4, D, 2048]); w_up_s = din("w_up_s", [4, D, 2048]); w_down_s = din("w_down_s", [4, 2048, D])
    ident_in = din("ident_in", [128, 128])
    ohE = din("ohE", [1, 64]); ohI = din("ohI", [1, 4]); lowmask = din("lowmask", [128, 128])
    iota512 = din("iota512", [128, 512]); tokid = din("tokid", [128, 32])
    ebase = din("ebase", [128, 16])
    out = nc.dram_tensor("out", [NTOK, D], F32, kind="ExternalOutput").ap()

    modg_in = dscr("modg_in", [2, 6144], F32); modg_out = dscr("modg_out", [8, 6144], F32)
    QaT = dscr("QaT", [16, 128, NTOK], BF16); KaT = dscr("KaT", [16, 128, NKV + NCTX], BF16)
    Va = dscr("Va", [NKV + NCTX, 2048], BF16)
    QrT = dscr("QrT", [8, 128, 2, NTOK], BF16); KrT = dscr("KrT", [8, 128, 2, NTOK], BF16)
    Kr = dscr("Kr", [NTOK, 2048], BF16); Vr = dscr("Vr", [NTOK, 2048], BF16)
    Krc = dscr("Krc", [NCTX, 2048], BF16); Vrc = dscr("Vrc", [NCTX, 2048], BF16)
    Grf = dscr("Grf", [NTOK, 2048], F32); Grb = dscr("Grb", [NTOK, 2048], F32)
    GaT = dscr("GaT", [32, 128, NTOK], F32); GbT = dscr("GbT", [32, 128, NTOK], F32)
    Lst_in = dscr("Lst_in", [16 * 2 * 128, 256], F32); Lst_out = dscr("Lst_out", [4 * 16 * 2 * 128, 256], F32)
    S0d = dscr("S0d", [16 * 2 * 128, 256], F32)
    ynaT = dscr("ynaT", [16, 128, NTOK], BF16); yretT = dscr("yretT", [16, 128, NTOK], BF16)
    Xmid = din("Xmid", [NTOK, D]) if XMID_IN else dscr("Xmid", [NTOK, D], F32)
    h2_in = dscr("h2_in", [NTOK, D], BF16); h2_all = dscr("h2_all", [4 * NTOK, D], BF16)
    aff_in = dscr("aff_in", [NTOK, 16], F32); aff_all = dscr("aff_all", [4 * NTOK, 16], F32)
    vsel = dscr("vsel", [4, 4 * NTOK], F32)
    rank_in = dscr("rank_in", [128, 128], F32); rank_all = dscr("rank_all", [4 * 128, 128], F32)
    ye_in = dscr("ye_in", [2048, D], BF16); ye_all = dscr("ye_all", [4 * 2048, D], BF16)

    G8 = [list(range(8))]
    G4 = [[0, 1, 2, 3], [4, 5, 6, 7]]

    with contextlib.ExitStack() as top:
        fw = FW(nc, top)
        nc._fw = fw
        R_scr = {}

        def rs(name):
            if name not in R_scr:
                R_scr[name] = Res(name, multi=True)
            return R_scr[name]

        uid = [0]

        def sb(st, name, shape, dt):
            uid[0] += 1
            return st.enter_context(nc.sbuf_tensor("%s_%d" % (name, uid[0]), list(shape), dt))

        def ps(st, name, shape, dt=F32):
            uid[0] += 1
            esz = 4 if dt == F32 else 2
            per_bank = 2048 // esz
            free = 1
            for d_ in shape[1:]:
                free *= d_
            nbank = (free + per_bank - 1) // per_bank
            t = st.enter_context(nc.psum_tensor("%s_%d" % (name, uid[0]), [128, nbank * per_bank], dt))
            return t[0:shape[0], 0:free]

        def allgather(src, dst, groups, rsrc, rdst):
            fw.custom("pool", lambda: nc.gpsimd.collective_compute(
                "AllGather", ALU.bypass, replica_groups=groups, ins=[src.opt()], outs=[dst.opt()]),
                1, reads=[rsrc], writes=[rdst])

        def allgather_chunks(src, dst, groups, nchunks, rsrc, rdst):
            nr = len(groups[0])
            rc = src.shape[0] // nchunks
            for c in range(nchunks):
                s_ap = src[c * rc:(c + 1) * rc, :]; d_ap = dst[c * nr * rc:(c + 1) * nr * rc, :]
                if src.dtype == BF16:
                    s_ap = s_ap.bitcast(F32); d_ap = d_ap.bitcast(F32)
                allgather(s_ap, d_ap, groups, rsrc, rdst)

        ident_bf = sb(top, "ident_bf", [128, 128], BF16); ident_f = sb(top, "ident_f", [128, 128], F32)
        r_ident = Res("ident")
        fw.dma("sp", ident_f[:], ident_in, writes=[r_ident])
        fw.op("dve", lambda: nc.vector.tensor_copy(out=ident_bf[:], in_=ident_f[:]), reads=[r_ident], writes=[r_ident])

        def mod_bc(dst, seg, row, r_dst, q="sp"):
            n0 = seg * 4096
            pos = 0
            while pos < 4096:
                n = n0 + pos
                i = n // 6144
                lo = n % 6144
                ln = min(4096 - pos, 6144 - lo)
                fw.dma(q, dst[:, pos:pos + ln], modg_out[i * 2 + row, lo:lo + ln].partition_broadcast(128),
                       reads=[rs("modg_out")], writes=[r_dst])
                pos += ln

        with contextlib.ExitStack() as st:
            cTf = sb(st, "cTf", [128, KC, 2], F32); cTb = sb(st, "cTb", [128, KC, 2], BF16); r_c = Res()
            wt = [sb(st, "m_wt%d" % i, [128, KC, 512], BF16) for i in range(2)]; r_wt = [Res(), Res()]
            bm = sb(st, "bm", [2, 6144], F32); r_bm = Res()
            mrow = sb(st, "mrow", [2, 6144], F32); r_mrow = Res()
            pm = Rot([ps(st, "pm%d" % i, [2, 512]) for i in range(2)])
            fw.dma("sp", cTf[:], cT, writes=[r_c])
            fw.dma("sp", bm[:], bmod_row[0, :].partition_broadcast(2), writes=[r_bm])
            fw.op("act", lambda: nc.scalar.activation(out=cTb[:], in_=cTf[:], func=AF.Silu), reads=[r_c], writes=[r_c])
            wv = w_mod_s.rearrange("(c p) n -> p c n", p=128)
            fw.dma("pool", wt[0][:], wv[:, :, 0:512], writes=[r_wt[0]])
            for b in range(12):
                if b + 1 < 12:
                    fw.dma("pool", wt[(b + 1) % 2][:], wv[:, :, (b + 1) * 512:(b + 2) * 512], writes=[r_wt[(b + 1) % 2]])
                p_t, p_r = pm.next()
                for c in range(KC):
                    fw.op("pe", lambda: nc.tensor.matmul(p_t[:], lhsT=cTb[:, c, :], rhs=wt[b % 2][:, c, :], start=(c == 0), stop=(c == KC - 1)),
                          reads=[r_c, r_wt[b % 2]], writes=[p_r], signal=(c == KC - 1))
                fw.op("dve", lambda: nc.vector.tensor_tensor(out=mrow[:, b * 512:(b + 1) * 512], in0=p_t[:], in1=bm[:, b * 512:(b + 1) * 512], op=ALU.add),
                      reads=[p_r, r_bm], writes=[r_mrow])
            fw.dma("sp", modg_in, mrow[:], reads=[r_mrow], writes=[rs("modg_in")])
            allgather(modg_in, modg_out, G4, rs("modg_in"), rs("modg_out"))
        fw.barrier()

        def build_hT(st_outer, xsrc, ntok, hT, r_hT, tok0, tabA, tabB, r_tab, tag, tm_out=None, src_res=None):
            with contextlib.ExitStack() as st:
                xt = Rot([sb(st, tag + "xt%d" % i, [128, D], F32) for i in range(2)])
                junk = sb(st, tag + "junk", [128, D], BF16); r_junk = Res()
                xs = Rot([sb(st, tag + "xs%d" % i, [128, D], BF16) for i in range(2)])
                stat = Rot([sb(st, tag + "stat%d" % i, [128, 4], F32) for i in range(2)])
                ptr = Rot([ps(st, tag + "ptr%d" % i, [128, 1024], BF16) for i in range(2)])
                xv = xsrc.rearrange("(t p) d -> t p d", p=128)
                nt = ntok // 128
                for t in range(nt):
                    x_t, x_r = xt.next()
                    fw.dma("sp", x_t[:], xv[t], reads=([src_res] if src_res is not None else []), writes=[x_r])
                    s_t, s_r = stat.next()
                    fw.op("act", lambda: nc.scalar.activation(out=junk[:], in_=x_t[:], func=AF.Square, accum_out=s_t[:, 0:1]),
                          reads=[x_r], writes=[r_junk, s_r])
                    fw.op("act", lambda: nc.scalar.activation(out=s_t[:, 1:2], in_=s_t[:, 0:1], func=AF.Ln, scale=1.0 / D, bias=EPS),
                          reads=[s_r], writes=[s_r])
                    fw.op("act", lambda: nc.scalar.activation(out=s_t[:, 2:3], in_=s_t[:, 1:2], func=AF.Exp, scale=-0.5),
                          reads=[s_r], writes=[s_r])
                    fw.op("dve", lambda: nc.vector.scalar_tensor_tensor(out=x_t[:], in0=x_t[:], scalar=s_t[:, 2:3], in1=tabA[:], op0=ALU.mult, op1=ALU.mult),
                          reads=[s_r, r_tab], writes=[x_r])
                    xs_t, xs_r = xs.next()
                    fw.op("pool", lambda: nc.gpsimd.tensor_tensor(out=xs_t[:], in0=x_t[:], in1=tabB[:], op=ALU.add),
                          reads=[x_r, r_tab], writes=[xs_r])
                    if tm_out is not None:
                        fw.dma("sp", tm_out[t * 128:(t + 1) * 128, :], xs_t[:], reads=[xs_r], writes=[rs("tm_out")])
                    for g in range(4):
                        p_t, p_r = ptr.next()
                        for k in range(8):
                            c = g * 8 + k
                            fw.op("pe", lambda: nc.tensor.transpose(out=p_t[:, k * 128:(k + 1) * 128], in_=xs_t[:, c * 128:(c + 1) * 128], identity=ident_bf[:]),
                                  reads=[xs_r, r_ident], writes=[p_r], signal=(k == 7))
                        dst = hT[:, g * 8:(g + 1) * 8, tok0 + t * 128: tok0 + (t + 1) * 128]
                        src = p_t[:].rearrange("p (k t) -> p k t", k=8)
                        if g % 2 == 0:
                            fw.op("act", lambda: nc.scalar.copy(out=dst, in_=src), reads=[p_r], writes=[r_hT])
                        else:
                            fw.op("dve", lambda: nc.vector.tensor_copy(out=dst, in_=src), reads=[p_r], writes=[r_hT])

        def load_tabs(st, tabA, tabB, r_tab, row, seg_sh, seg_sc, nrow):
            with contextlib.ExitStack() as st2:
                nb = sb(st2, "nb_tmp", [128, D], F32); r_nb = Res()
                fw.dma("sp", nb[:], nrow[0, :].partition_broadcast(128), writes=[r_nb])
                mod_bc(tabA, seg_sc, row, r_tab)
                mod_bc(tabB, seg_sh, row, r_tab)
                fw.op("dve", lambda: nc.vector.scalar_tensor_tensor(out=tabA[:], in0=tabA[:], scalar=1.0, in1=nb[:], op0=ALU.add, op1=ALU.mult),
                      reads=[r_nb, r_tab], writes=[r_tab])
                fw.barrier()

        def stream_weights(wsrc_fn, nblocks, wt, r_wt, compute_fn):
            fw.dma("pool", wt[0][:], wsrc_fn(0), writes=[r_wt[0]])
            for b in range(nblocks):
                if b + 1 < nblocks:
                    s = (b + 1) % 2
                    fw.dma("pool", wt[s][:], wsrc_fn(b + 1), writes=[r_wt[s]])
                compute_fn(b, wt[b % 2], r_wt[b % 2])

        def mm_fm(p_t, p_r, w_t, w_r, wcol0, hT, r_hT, t0, tl, kc=KC):
            for c in range(kc):
                fw.op("pe", lambda: nc.tensor.matmul(p_t[:, 0:tl], lhsT=w_t[:, c, wcol0:wcol0 + 128], rhs=hT[:, c, t0:t0 + tl], start=(c == 0), stop=(c == kc - 1)),
                      reads=[w_r, r_hT], writes=[p_r], signal=(c == kc - 1))

        def mm_tm(p_t, p_r, w_t, w_r, wcol0, wn, hT, r_hT, t0, kc=KC):
            for c in range(kc):
                fw.op("pe", lambda: nc.tensor.matmul(p_t[:, 0:wn], lhsT=hT[:, c, t0:t0 + 128], rhs=w_t[:, c, wcol0:wcol0 + wn], start=(c == 0), stop=(c == kc - 1)),
                      reads=[w_r, r_hT], writes=[p_r], signal=(c == kc - 1))

        w_in_v = w_in.rearrange("(c p) n -> p c n", p=128) if w_in is not None else None
        aff_own = sb(top, "aff_own", [128, 8, 16], F32); r_aff = Res("aff_own")

        if "P1" in PHASES:
            with contextlib.ExitStack() as st:
                hT = sb(st, "hT_kvc", [128, KC, NKV + NCTX], BF16); r_hT = Res()
                with contextlib.ExitStack() as st1:
                    tabA = sb(st1, "tabA", [128, D], F32); tabB = sb(st1, "tabB", [128, D], F32); r_tab = Res()
                    load_tabs(st1, tabA, tabB, r_tab, 1, 0, 1, norm1_row)
                    build_hT(st1, ctxb, NCTX, hT, r_hT, NKV, tabA, tabB, r_tab, "c_")
                    fw.barrier()
                    load_tabs(st1, tabA, tabB, r_tab, 0, 0, 1, norm1_row)
                    build_hT(st1, x_kv, NKV, hT, r_hT, 0, tabA, tabB, r_tab, "k_")
                fw.barrier()
                WB = 256
                wt = [sb(st, "p1_wt%d" % i, [128, KC, WB], BF16) for i in range(2)]; r_wt = [Res(), Res()]
                pp = Rot([ps(st, "p1_ps%d" % i, [128, 512]) for i in range(4)])
                og = Rot([sb(st, "p1_o%d" % i, [128, 512], BF16) for i in range(4)])
                NT1 = NKV + NCTX
                blocks = [("nak", 2048 + i * WB) for i in range(2048 // WB)] + [("nav", 4096 + i * WB) for i in range(2048 // WB)] + \
                         [("rk", 8192 + i * WB) for i in range(2048 // WB)] + [("rv", 10240 + i * WB) for i in range(2048 // WB)]

                def p1_compute(b, w_t, w_r):
                    fam, col0 = blocks[b]
                    if fam == "nak":
                        for sub in range(WB // 128):
                            hd = (col0 - 2048) // 128 + sub
                            for t0 in range(0, NT1, 512):
                                tl = min(512, NT1 - t0)
                                p_t, p_r = pp.next()
                                mm_fm(p_t, p_r, w_t, w_r, sub * 128, hT, r_hT, t0, tl)
                                o_t, o_r = og.next()
                                fw.op("act", lambda: nc.scalar.copy(out=o_t[:, 0:tl], in_=p_t[:, 0:tl]), reads=[p_r], writes=[o_r])
                                fw.dma("sp", KaT[hd, :, t0:t0 + tl], o_t[:, 0:tl], reads=[o_r], writes=[rs("KaT")])
                    else:
                        toks = range(0, NT1, 128) if fam == "nav" else range(NKV, NT1, 128)
                        for t0 in toks:
                            p_t, p_r = pp.next()
                            mm_tm(p_t, p_r, w_t, w_r, 0, WB, hT, r_hT, t0)
                            o_t, o_r = og.next()
                            if fam == "rk":
                                fw.op("act", lambda: nc.scalar.mul(out=o_t[:, 0:WB], in_=p_t[:, 0:WB], mul=1.0 / 16.0), reads=[p_r], writes=[o_r])
                            else:
                                fw.op("dve", lambda: nc.vector.tensor_copy(out=o_t[:, 0:WB], in_=p_t[:, 0:WB]), reads=[p_r], writes=[o_r])
                            if fam == "nav":
                                fw.dma("sp", Va[t0:t0 + 128, col0 - 4096:col0 - 4096 + WB], o_t[:, 0:WB], reads=[o_r], writes=[rs("Va")])
                            elif fam == "rk":
                                fw.dma("sp", Krc[t0 - NKV:t0 - NKV + 128, col0 - 8192:col0 - 8192 + WB], o_t[:, 0:WB], reads=[o_r], writes=[rs("Krc")])
                            else:
                                fw.dma("sp", Vrc[t0 - NKV:t0 - NKV + 128, col0 - 10240:col0 - 10240 + WB], o_t[:, 0:WB], reads=[o_r], writes=[rs("Vrc")])

                stream_weights(lambda b: w_in_v[:, :, blocks[b][1]:blocks[b][1] + WB], len(blocks), wt, r_wt, p1_compute)
            fw.barrier()

        if "P2" in PHASES:
            with contextlib.ExitStack() as st:
                hT = sb(st, "hT_own", [128, KC, NTOK], BF16); r_hT = Res()
                with contextlib.ExitStack() as st1:
                    tabA = sb(st1, "tabA2", [128, D], F32); tabB = sb(st1, "tabB2", [128, D], F32); r_tab = Res()
                    load_tabs(st1, tabA, tabB, r_tab, 0, 0, 1, norm1_row)
                    build_hT(st1, x_own, NTOK, hT, r_hT, 0, tabA, tabB, r_tab, "o_")
                fw.barrier()
                WB = 512
                wt = [sb(st, "p2_wt%d" % i, [128, KC, WB], BF16) for i in range(2)]; r_wt = [Res(), Res()]
                pp = Rot([ps(st, "p2_ps%d" % i, [128, 512]) for i in range(4)])
                ptr = Rot([ps(st, "p2_ptr%d" % i, [128, 512], BF16) for i in range(2)])
                ob = Rot([sb(st, "p2_ob%d" % i, [128, 512], BF16) for i in range(4)])
                of = Rot([sb(st, "p2_of%d" % i, [128, 512], F32) for i in range(4)])
                otr = Rot([sb(st, "p2_otr%d" % i, [128, 512], BF16) for i in range(2)])
                tmp = Rot([sb(st, "p2_tmp%d" % i, [128, 4, 128], F32) for i in range(2)])
                rc = sb(st, "ropeC_sb", [128, 8, 128], F32); rsn = sb(st, "ropeS_sb", [128, 8, 128], F32)
                rck = sb(st, "ropeCk_sb", [128, 8, 128], F32); rsk = sb(st, "ropeSk_sb", [128, 8, 128], F32); r_rope = Res()
                for tsb, src in ((rc, ropeC), (rsn, ropeS), (rck, ropeCk), (rsk, ropeSk)):
                    fw.dma("sp", tsb[:], src.rearrange("(t p) f -> p t f", p=128), writes=[r_rope])
                blocks = [("naq", i * WB) for i in range(2048 // WB)] + [("rq", 6144 + i * WB) for i in range(2048 // WB)] + \
                         [("rk", 8192 + i * WB) for i in range(2048 // WB)] + [("rv", 10240 + i * WB) for i in range(2048 // WB)] + \
                         [("grf", 12288 + i * WB) for i in range(2048 // WB)] + [("grb", 14336 + i * WB) for i in range(2048 // WB)] + \
                         [("ga", 16384 + i * WB) for i in range(4096 // WB)] + [("gb", 20480 + i * WB) for i in range(4096 // WB)]

                def rope(p_t, p_r, tt, o_t, o_r, ctab, stab):
                    t_t, t_r = tmp.next()
                    for hh in range(2):
                        a = p_t[:, hh * 256:(hh + 1) * 256].rearrange("p (r h f) -> p r h f", r=2, h=2)
                        o = o_t[:, hh * 256:(hh + 1) * 256].rearrange("p (r h f) -> p r h f", r=2, h=2)
                        cs = ctab[:, tt, :].rearrange("p (r f) -> p r f", r=2)
                        sn = stab[:, tt, :].rearrange("p (r f) -> p r f", r=2)
                        a1 = a[:, :, 0, :]; a2 = a[:, :, 1, :]
                        T = [t_t[:, k, :].rearrange("p (r f) -> p r f", r=2) for k in range(4)]
                        fw.op("dve", lambda: nc.vector.tensor_tensor(out=T[0], in0=a1, in1=cs, op=ALU.mult), reads=[p_r, r_rope], writes=[t_r])
                        fw.op("dve", lambda: nc.vector.tensor_tensor(out=T[1], in0=a2, in1=sn, op=ALU.mult), reads=[p_r, r_rope], writes=[t_r])
                        fw.op("dve", lambda: nc.vector.tensor_tensor(out=T[2], in0=a1, in1=sn, op=ALU.mult), reads=[p_r, r_rope], writes=[t_r])
                        fw.op("dve", lambda: nc.vector.tensor_tensor(out=T[3], in0=a2, in1=cs, op=ALU.mult), reads=[p_r, r_rope], writes=[t_r])
                        fw.op("pool", lambda: nc.gpsimd.tensor_tensor(out=o[:, :, 0, :], in0=T[0], in1=T[1], op=ALU.subtract), reads=[t_r], writes=[o_r])
                        fw.op("pool", lambda: nc.gpsimd.tensor_tensor(out=o[:, :, 1, :], in0=T[2], in1=T[3], op=ALU.add), reads=[t_r], writes=[o_r])

                def p2_compute(b, w_t, w_r):
                    fam, col0 = blocks[b]
                    if fam in ("naq", "ga", "gb"):
                        for sub in range(WB // 128):
                            for t0 in range(0, NTOK, 512):
                                p_t, p_r = pp.next()
                                mm_fm(p_t, p_r, w_t, w_r, sub * 128, hT, r_hT, t0, 512)
                                if fam == "naq":
                                    hd = col0 // 128 + sub
                                    o_t, o_r = ob.next()
                                    fw.op("act", lambda: nc.scalar.copy(out=o_t[:], in_=p_t[:]), reads=[p_r], writes=[o_r])
                                    fw.dma("sp", QaT[hd, :, t0:t0 + 512], o_t[:], reads=[o_r], writes=[rs("QaT")])
                                else:
                                    base = 16384 if fam == "ga" else 20480
                                    dst = GaT if fam == "ga" else GbT
                                    ch = (col0 - base) // 128 + sub
                                    o_t, o_r = of.next()
                                    fw.op("act", lambda: nc.scalar.activation(out=o_t[:], in_=p_t[:], func=AF.Sigmoid), reads=[p_r], writes=[o_r])
                                    fw.dma("sp", dst[ch, :, t0:t0 + 512], o_t[:], reads=[o_r], writes=[rs("GaT" if fam == "ga" else "GbT")])
                    else:
                        for tt in range(NTOK // 128):
                            t0 = tt * 128
                            p_t, p_r = pp.next()
                            mm_tm(p_t, p_r, w_t, w_r, 0, WB, hT, r_hT, t0)
                            if fam in ("rq", "rk"):
                                o_t, o_r = ob.next()
                                base = 6144 if fam == "rq" else 8192
                                rope(p_t, p_r, tt, o_t, o_r, rc if fam == "rq" else rck, rsn if fam == "rq" else rsk)
                                c0 = col0 - base
                                if fam == "rk":
                                    fw.dma("sp", Kr[t0:t0 + 128, c0:c0 + WB], o_t[:], reads=[o_r], writes=[rs("Kr")])
                                q_t, q_r = ptr.next()
                                for k in range(4):
                                    fw.op("pe", lambda: nc.tensor.transpose(out=q_t[:, k * 128:(k + 1) * 128], in_=o_t[:, k * 128:(k + 1) * 128], identity=ident_bf[:]),
                                          reads=[o_r, r_ident], writes=[q_r], signal=(k == 3))
                                x_t, x_r = otr.next()
                                fw.op("act", lambda: nc.scalar.copy(out=x_t[:], in_=q_t[:]), reads=[q_r], writes=[x_r])
                                dstT = QrT if fam == "rq" else KrT
                                h0 = c0 // 256
                                for hh in range(2):
                                    fw.dma("sp", dstT[h0 + hh, :, :, t0:t0 + 128],
                                           x_t[:, hh * 256:(hh + 1) * 256].rearrange("p (c t) -> p c t", c=2), reads=[x_r], writes=[rs("QrT" if fam == "rq" else "KrT")])
                            elif fam == "rv":
                                o_t, o_r = ob.next()
                                fw.op("act", lambda: nc.scalar.copy(out=o_t[:], in_=p_t[:]), reads=[p_r], writes=[o_r])
                                fw.dma("sp", Vr[t0:t0 + 128, col0 - 10240:col0 - 10240 + WB], o_t[:], reads=[o_r], writes=[rs("Vr")])
                            else:
                                o_t, o_r = of.next()
                                fw.op("act", lambda: nc.scalar.activation(out=o_t[:], in_=p_t[:], func=AF.Silu), reads=[p_r], writes=[o_r])
                                base = 12288 if fam == "grf" else 14336
                                dst = Grf if fam == "grf" else Grb
                                fw.dma("sp", dst[t0:t0 + 128, col0 - base:col0 - base + WB], o_t[:], reads=[o_r], writes=[rs("Grf" if fam == "grf" else "Grb")])

                stream_weights(lambda b: w_in_v[:, :, blocks[b][1]:blocks[b][1] + WB], len(blocks), wt, r_wt, p2_compute)
            fw.barrier()


        if "NA" in PHASES:
            with contextlib.ExitStack() as st:
                oneA = sb(st, "oneA", [128, NKV], BF16); vB = sb(st, "vB", [128, NTOK], BF16); r_mask = Res()
                fw.dma("pool", oneA[:], onehotA, writes=[r_mask]); fw.dma("pool", vB[:], validB, writes=[r_mask])
                ones_bf = sb(st, "ones_bf", [128, 128], BF16); r_ones = Res()
                fw.op("dve", lambda: nc.vector.memset(ones_bf[:], 1.0), writes=[r_ones])
                kTr = Rot([sb(st, "na_kT%d" % i, [128, NKV + NCTX], BF16) for i in range(2)])
                vAr = Rot([sb(st, "na_vA%d" % i, [128, 14, 128], BF16) for i in range(2)])
                qTr = Rot([sb(st, "na_qT%d" % i, [128, NTOK], BF16) for i in range(2)])
                bfr = Rot([sb(st, "na_bf%d" % i, [128, 896], F32) for i in range(2)])
                ebr = Rot([sb(st, "na_eb%d" % i, [128, 896], BF16) for i in range(2)])
                yTr = Rot([sb(st, "na_yT%d" % i, [128, NTOK], BF16) for i in range(2)])
                Pr = Rot([sb(st, "na_P%d" % i, [128, 1024], BF16) for i in range(2)])
                rdr = Rot([sb(st, "na_rd%d" % i, [128, 128], F32) for i in range(2)])
                Sps = Rot([ps(st, "na_S%d" % i, [128, 1024]) for i in range(2)])
                Ops = Rot([ps(st, "na_O%d" % i, [128, 256]) for i in range(2)])
                Va_v = Va.rearrange("(t p) c -> p t c", p=128)
                SC = 128 ** -0.5
                for h in range(16):
                    kT, kT_r = kTr.next(); vA, vA_r = vAr.next(); qT, qT_r = qTr.next()
                    bf, bf_r = bfr.next(); eb, eb_r = ebr.next(); yT, yT_r = yTr.next()
                    fw.dma("sp", kT[:], KaT[h], reads=[rs("KaT")], writes=[kT_r])
                    fw.dma("sp", vA[:], Va_v[:, :, h * 128:(h + 1) * 128], reads=[rs("Va")], writes=[vA_r])
                    fw.dma("sp", qT[:], QaT[h], reads=[rs("QaT")], writes=[qT_r])
                    fw.dma("sp", bf[:], nabias[h], writes=[bf_r])
                    fw.op("act", lambda: nc.scalar.activation(out=eb[:], in_=bf[:], func=AF.Exp), reads=[bf_r], writes=[eb_r])
                    for pr in range(8):
                        if pr == 0:
                            kts = list(range(0, 6)); idx0 = 1
                        elif pr == 7:
                            kts = list(range(6, 12)); idx0 = 0
                        else:
                            kts = list(range(pr, pr + 5)); idx0 = 1
                        nw = len(kts); nt = nw + 2
                        S_t, S_r = Sps.next()
                        q_ap = qT[:, pr * 128:(pr + 1) * 128]
                        for i, kt in enumerate(kts):
                            fw.op("pe", lambda: nc.tensor.matmul(S_t[:, i * 128:(i + 1) * 128], lhsT=kT[:, kt * 128:(kt + 1) * 128], rhs=q_ap, start=True, stop=False),
                                  reads=[kT_r, qT_r], writes=[S_r], signal=False)
                            fw.op("pe", lambda: nc.tensor.matmul(S_t[:, i * 128:(i + 1) * 128], lhsT=oneA[:, kt * 128:(kt + 1) * 128], rhs=vB[:, pr * 128:(pr + 1) * 128], start=False, stop=True),
                                  reads=[r_mask], writes=[S_r], signal=False)
                        for i in range(2):
                            fw.op("pe", lambda: nc.tensor.matmul(S_t[:, (nw + i) * 128:(nw + i + 1) * 128], lhsT=kT[:, NKV + i * 128:NKV + (i + 1) * 128], rhs=q_ap, start=True, stop=True),
                                  reads=[kT_r, qT_r], writes=[S_r], signal=(i == 1))
                        P_t, P_r = Pr.next()
                        fw.op("act", lambda: nc.scalar.activation(out=P_t[:, 0:512], in_=S_t[:, 0:512], func=AF.Exp, scale=SC), reads=[S_r], writes=[P_r])
                        fw.op("act", lambda: nc.scalar.activation(out=P_t[:, 512:nt * 128], in_=S_t[:, 512:nt * 128], func=AF.Exp, scale=SC), reads=[S_r], writes=[P_r])
                        fw.op("dve", lambda: nc.vector.tensor_tensor(out=P_t[:, 0:nw * 128], in0=P_t[:, 0:nw * 128], in1=eb[:, idx0 * 128:(idx0 + nw) * 128], op=ALU.mult),
                              reads=[eb_r, P_r], writes=[P_r])
                        O_t, O_r = Ops.next()
                        vts = kts + [12, 13]
                        for i in range(nt):
                            fw.op("pe", lambda: nc.tensor.matmul(O_t[:, 0:128], lhsT=vA[:, vts[i], :], rhs=P_t[:, i * 128:(i + 1) * 128], start=(i == 0), stop=(i == nt - 1)),
                                  reads=[vA_r, P_r], writes=[O_r], signal=False)
                        for i in range(nt):
                            fw.op("pe", lambda: nc.tensor.matmul(O_t[:, 128:256], lhsT=ones_bf[:], rhs=P_t[:, i * 128:(i + 1) * 128], start=(i == 0), stop=(i == nt - 1)),
                                  reads=[r_ones, P_r], writes=[O_r], signal=(i == nt - 1))
                        rd, rd_r = rdr.next()
                        fw.op("dve", lambda: nc.vector.reciprocal(out=rd[:], in_=O_t[:, 128:256]), reads=[O_r], writes=[rd_r])
                        fw.op("dve", lambda: nc.vector.tensor_tensor(out=yT[:, pr * 128:(pr + 1) * 128], in0=O_t[:, 0:128], in1=rd[:], op=ALU.mult),
                              reads=[O_r, rd_r], writes=[yT_r])
                    fw.dma("sp", ynaT[h], yT[:], reads=[yT_r], writes=[rs("ynaT")])
            fw.barrier()

        if "R" in PHASES:
            with contextlib.ExitStack() as st:
                rd16 = sb(st, "rd16", [128, 16], F32); lg = sb(st, "lg", [128, 16], F32); r_lg = Res()
                Mt = sb(st, "Mt", [128, 256], F32); pos = sb(st, "pos", [128, 26], F32); r_tb = Res()
                cE = sb(st, "cE", [128, 10], F32); cM = sb(st, "cM", [128, 10], F32)
                Dt = sb(st, "Dt", [128, 16, 128], F32); ex = sb(st, "ex", [128, 16, 13], F32); cco = sb(st, "cco", [128, 16, 5], F32); r_ex = Res()
                fw.dma("sp", rd16[:], ret_decay[0, :].partition_broadcast(128), writes=[r_lg])
                fw.dma("sp", Mt[:], retM, writes=[r_tb]); fw.dma("sp", pos[:], retpos, writes=[r_tb])
                fw.dma("sp", cE[:], ccE[0, :].partition_broadcast(128), writes=[r_tb]); fw.dma("sp", cM[:], ccM[0, :].partition_broadcast(128), writes=[r_tb])
                fw.op("act", lambda: nc.scalar.activation(out=rd16[:], in_=rd16[:], func=AF.Exp, scale=-0.6931471805599453), reads=[r_lg], writes=[r_lg])
                fw.op("act", lambda: nc.scalar.activation(out=lg[:], in_=rd16[:], func=AF.Ln, scale=-1.0, bias=1.0), reads=[r_lg], writes=[r_lg])
                for k in range(16):
                    d = k // 8
                    fw.op("dve", lambda: nc.vector.tensor_scalar(out=Dt[:, k, :], in0=Mt[:, d * 128:(d + 1) * 128], scalar1=lg[:, k:k + 1], scalar2=None, op0=ALU.mult),
                          reads=[r_lg, r_tb], writes=[r_ex])
                    fw.op("dve", lambda: nc.vector.tensor_scalar(out=ex[:, k, :], in0=pos[:, d * 13:(d + 1) * 13], scalar1=lg[:, k:k + 1], scalar2=None, op0=ALU.mult),
                          reads=[r_lg, r_tb], writes=[r_ex])
                    fw.op("dve", lambda: nc.vector.tensor_scalar(out=cco[:, k, :], in0=cE[:, d * 5:(d + 1) * 5], scalar1=lg[:, k:k + 1], scalar2=None, op0=ALU.mult),
                          reads=[r_lg, r_tb], writes=[r_ex])
                fw.op("act", lambda: nc.scalar.activation(out=Dt[:], in_=Dt[:], func=AF.Exp), reads=[r_ex], writes=[r_ex])
                fw.op("act", lambda: nc.scalar.activation(out=ex[:], in_=ex[:], func=AF.Exp), reads=[r_ex], writes=[r_ex])
                fw.op("act", lambda: nc.scalar.activation(out=cco[:], in_=cco[:], func=AF.Exp), reads=[r_ex], writes=[r_ex])
                for k in range(16):
                    d = k // 8
                    fw.op("dve", lambda: nc.vector.tensor_tensor(out=cco[:, k, :], in0=cco[:, k, :], in1=cM[:, d * 5:(d + 1) * 5], op=ALU.mult), reads=[r_ex, r_tb], writes=[r_ex])
                Kr_v = Kr.rearrange("(t p) c -> p t c", p=128); Vr_v = Vr.rearrange("(t p) c -> p t c", p=128)
                Krc_v = Krc.rearrange("(t p) c -> p t c", p=128); Vrc_v = Vrc.rearrange("(t p) c -> p t c", p=128)
                Grf_v = Grf.rearrange("(t p) c -> p t c", p=128); Grb_v = Grb.rearrange("(t p) c -> p t c", p=128)
                Sf = sb(st, "Sf", [128, 32, 256], F32); Sb_ = sb(st, "Sbf", [128, 32, 256], BF16)
                r_S = [Res() for _ in range(32)]
                with contextlib.ExitStack() as st1:
                    Khr = Rot([sb(st1, "r1_K%d" % i, [128, 8, 256], BF16) for i in range(2)])
                    Vhr = Rot([sb(st1, "r1_V%d" % i, [128, 8, 256], BF16) for i in range(2)])
                    Kcr = Rot([sb(st1, "r1_Kc%d" % i, [128, 2, 256], BF16) for i in range(2)])
                    Vcr = Rot([sb(st1, "r1_Vc%d" % i, [128, 2, 256], BF16) for i in range(2)])
                    Kwr = Rot([sb(st1, "r1_Kw%d" % i, [128, 10, 256], BF16) for i in range(2)])
                    Lor = Rot([sb(st1, "r1_Lo%d" % i, [128, 256], F32) for i in range(4)])
                    Lps = Rot([ps(st1, "r1_ps%d" % i, [128, 256]) for i in range(4)])
                    for h in range(8):
                        Kh, Kh_r = Khr.next(); Vh, Vh_r = Vhr.next(); Kc, Kc_r = Kcr.next(); Vc, Vc_r = Vcr.next()
                        fw.dma("sp", Kh[:], Kr_v[:, :, h * 256:(h + 1) * 256], reads=[rs("Kr")], writes=[Kh_r])
                        fw.dma("sp", Vh[:], Vr_v[:, :, h * 256:(h + 1) * 256], reads=[rs("Vr")], writes=[Vh_r])
                        fw.dma("sp", Kc[:], Krc_v[:, :, h * 256:(h + 1) * 256], reads=[rs("Krc")], writes=[Kc_r])
                        fw.dma("sp", Vc[:], Vrc_v[:, :, h * 256:(h + 1) * 256], reads=[rs("Vrc")], writes=[Vc_r])
                        for d in range(2):
                            k = d * 8 + h
                            Kw, Kw_r = Kwr.next()
                            for t in range(8):
                                eng = "dve" if t % 2 == 0 else "pool"
                                E_ = nc.vector if eng == "dve" else nc.gpsimd
                                fw.op(eng, lambda: E_.tensor_scalar(out=Kw[:, t, :], in0=Kh[:, t, :], scalar1=ex[:, k, 3 + t:4 + t], scalar2=None, op0=ALU.mult),
                                      reads=[Kh_r, r_ex], writes=[Kw_r])
                            for t in range(2):
                                fw.op("dve", lambda: nc.vector.tensor_scalar(out=Kw[:, 8 + t, :], in0=Kc[:, t, :], scalar1=ex[:, k, 11 + t:12 + t], scalar2=None, op0=ALU.mult),
                                      reads=[Kc_r, r_ex], writes=[Kw_r])
                            for dkc in range(2):
                                row0 = (k * 2 + dkc) * 128
                                p_t, p_r = Lps.next()
                                for t in range(8):
                                    fw.op("pe", lambda: nc.tensor.matmul(p_t[:], lhsT=Kw[:, t, dkc * 128:(dkc + 1) * 128], rhs=Vh[:, t, :], start=(t == 0), stop=(t == 7)),
                                          reads=[Kw_r, Vh_r], writes=[p_r], signal=(t == 7))
                                o_t, o_r = Lor.next()
                                fw.op("act", lambda: nc.scalar.copy(out=o_t[:], in_=p_t[:]), reads=[p_r], writes=[o_r])
                                fw.dma("sp", Lst_in[row0:row0 + 128, :], o_t[:], reads=[o_r], writes=[rs("Lst_in")])
                                p_t, p_r = Lps.next()
                                for t in range(2):
                                    fw.op("pe", lambda: nc.tensor.matmul(p_t[:], lhsT=Kw[:, 8 + t, dkc * 128:(dkc + 1) * 128], rhs=Vc[:, t, :], start=(t == 0), stop=(t == 1)),
                                          reads=[Kw_r, Vc_r], writes=[p_r], signal=(t == 1))
                                o_t, o_r = Lor.next()
                                fw.op("act", lambda: nc.scalar.copy(out=o_t[:], in_=p_t[:]), reads=[p_r], writes=[o_r])
                                fw.dma("sp", S0d[row0:row0 + 128, :], o_t[:], reads=[o_r], writes=[rs("S0d")])
                    allgather_chunks(Lst_in, Lst_out, G4, 4, rs("Lst_in"), rs("Lst_out"))
                    Lar = Rot([sb(st1, "r2_La%d" % i, [128, 5, 256], F32) for i in range(2)])
                    Lo_v = Lst_out.rearrange("(c i q p) v -> p c i q v", c=4, i=4, p=128)
                    for k in range(16):
                        for dkc in range(2):
                            q = k * 2 + dkc
                            La, La_r = Lar.next()
                            fw.dma("sp", La[:, 0:4, :], Lo_v[:, q // 8, :, q % 8, :], reads=[rs("Lst_out")], writes=[La_r])
                            fw.dma("sp", La[:, 4, :], S0d[q * 128:(q + 1) * 128, :], reads=[rs("S0d")], writes=[La_r])
                            fw.op("dve", lambda: nc.vector.tensor_scalar(out=Sf[:, q, :], in0=La[:, 0, :], scalar1=cco[:, k, 0:1], scalar2=None, op0=ALU.mult),
                                  reads=[La_r, r_ex], writes=[r_S[q]])
                            for i in range(1, 5):
                                fw.op("dve", lambda: nc.vector.scalar_tensor_tensor(out=Sf[:, q, :], in0=La[:, i, :], scalar=cco[:, k, i:i + 1], in1=Sf[:, q, :], op0=ALU.mult, op1=ALU.add),
                                      reads=[La_r, r_ex], writes=[r_S[q]])
                            fw.op("act", lambda: nc.scalar.copy(out=Sb_[:, q, :], in_=Sf[:, q, :]), reads=[r_S[q]], writes=[r_S[q]])
                fw.barrier()
                with contextlib.ExitStack() as st1:
                    QTr = Rot([sb(st1, "r3_QT%d" % i, [128, 2, NTOK], BF16) for i in range(2)])
                    KTr = Rot([sb(st1, "r3_KT%d" % i, [128, 2, NTOK], BF16) for i in range(2)])
                    Khr = Rot([sb(st1, "r3_K%d" % i, [128, 8, 256], BF16) for i in range(2)])
                    Vhr = Rot([sb(st1, "r3_V%d" % i, [128, 8, 256], BF16) for i in range(2)])
                    Gfr = Rot([sb(st1, "r3_Gf%d" % i, [128, 8, 256], F32) for i in range(2)])
                    Gbr = Rot([sb(st1, "r3_Gb%d" % i, [128, 8, 256], F32) for i in range(2)])
                    yaccr = Rot([sb(st1, "r3_ya%d" % i, [128, 8, 256], F32) for i in range(2)])
                    ybfr = Rot([sb(st1, "r3_yb%d" % i, [128, 8, 256], BF16) for i in range(2)])
                    Ar = Rot([sb(st1, "r3_A%d" % i, [128, 128], BF16) for i in range(2)])
                    o1r = Rot([sb(st1, "r3_o1%d" % i, [128, 256], F32) for i in range(2)])
                    osr = Rot([sb(st1, "r3_os%d" % i, [128, 256], F32) for i in range(2)])
                    tmr = Rot([sb(st1, "r3_tm%d" % i, [128, 256], F32) for i in range(2)])
                    jkr = Rot([sb(st1, "r3_jk%d" % i, [128, 256], BF16) for i in range(2)])
                    str_ = Rot([sb(st1, "r3_st%d" % i, [128, 4], F32) for i in range(4)])
                    Kdr = Rot([sb(st1, "r3_Kd%d" % i, [128, 256], BF16) for i in range(2)])
                    yor = Rot([sb(st1, "r3_yo%d" % i, [128, NTOK], BF16) for i in range(2)])
                    pA = Rot([ps(st1, "r3_pA%d" % i, [128, 128]) for i in range(2)])
                    pO = Rot([ps(st1, "r3_pO%d" % i, [128, 256]) for i in range(3)])
                    pS = Rot([ps(st1, "r3_pS%d" % i, [128, 256]) for i in range(2)])
                    pT = Rot([ps(st1, "r3_pT%d" % i, [128, NTOK], BF16) for i in range(1)])
                    for h in range(8):
                        QT, QT_r = QTr.next(); KT, KT_r = KTr.next(); Kh, Kh_r = Khr.next(); Vh, Vh_r = Vhr.next()
                        Gf, Gf_r = Gfr.next(); Gb, Gb_r = Gbr.next(); ya, ya_r = yaccr.next()
                        fw.dma("sp", QT[:], QrT[h], reads=[rs("QrT")], writes=[QT_r])
                        fw.dma("sp", KT[:], KrT[h], reads=[rs("KrT")], writes=[KT_r])
                        fw.dma("sp", Kh[:], Kr_v[:, :, h * 256:(h + 1) * 256], reads=[rs("Kr")], writes=[Kh_r])
                        fw.dma("sp", Vh[:], Vr_v[:, :, h * 256:(h + 1) * 256], reads=[rs("Vr")], writes=[Vh_r])
                        fw.dma("sp", Gf[:], Grf_v[:, :, h * 256:(h + 1) * 256], reads=[rs("Grf")], writes=[Gf_r])
                        fw.dma("sp", Gb[:], Grb_v[:, :, h * 256:(h + 1) * 256], reads=[rs("Grb")], writes=[Gb_r])
                        for d in range(2):
                            k = d * 8 + h
                            order = list(range(8)) if d == 0 else list(range(7, -1, -1))
                            G_, G_r = (Gf, Gf_r) if d == 0 else (Gb, Gb_r)
                            for oi, n in enumerate(order):
                                ch = slice(n * 128, (n + 1) * 128)
                                a_t, a_r = pA.next()
                                for dkc in range(2):
                                    fw.op("pe", lambda: nc.tensor.matmul(a_t[:], lhsT=KT[:, dkc, ch], rhs=QT[:, dkc, ch], start=(dkc == 0), stop=(dkc == 1)),
                                          reads=[KT_r, QT_r], writes=[a_r], signal=(dkc == 1))
                                A_t, A_r = Ar.next()
                                fw.op("dve", lambda: nc.vector.tensor_tensor(out=A_t[:], in0=a_t[:], in1=Dt[:, k, :], op=ALU.mult), reads=[a_r, r_ex], writes=[A_r])
                                o1_t, o1_r = pO.next()
                                fw.op("pe", lambda: nc.tensor.matmul(o1_t[:], lhsT=A_t[:], rhs=Vh[:, n, :], start=True, stop=True), reads=[A_r, Vh_r], writes=[o1_r])
                                o2_t, o2_r = pO.next()
                                for dkc in range(2):
                                    q = k * 2 + dkc
                                    fw.op("pe", lambda: nc.tensor.matmul(o2_t[:], lhsT=QT[:, dkc, ch], rhs=Sb_[:, q, :], start=(dkc == 0), stop=(dkc == 1)),
                                          reads=[QT_r, r_S[q]], writes=[o2_r], signal=(dkc == 1))
                                o1s, o1s_r = o1r.next()
                                fw.op("act", lambda: nc.scalar.copy(out=o1s[:], in_=o1_t[:]), reads=[o1_r], writes=[o1s_r])
                                os_, os_r = osr.next()
                                fw.op("dve", lambda: nc.vector.scalar_tensor_tensor(out=os_[:], in0=o2_t[:], scalar=ex[:, k, 0:1], in1=o1s[:], op0=ALU.mult, op1=ALU.add),
                                      reads=[o2_r, o1s_r, r_ex], writes=[os_r])
                                s_t, s_r = str_.next(); jk, jk_r = jkr.next()
                                fw.op("act", lambda: nc.scalar.activation(out=jk[:], in_=os_[:], func=AF.Square, accum_out=s_t[:, 0:1]), reads=[os_r], writes=[jk_r, s_r])
                                fw.op("act", lambda: nc.scalar.activation(out=s_t[:, 1:2], in_=s_t[:, 0:1], func=AF.Ln, scale=1.0 / 256, bias=EPS), reads=[s_r], writes=[s_r])
                                fw.op("act", lambda: nc.scalar.activation(out=s_t[:, 2:3], in_=s_t[:, 1:2], func=AF.Exp, scale=-0.5), reads=[s_r], writes=[s_r])
                                if d == 0:
                                    fw.op("dve", lambda: nc.vector.scalar_tensor_tensor(out=ya[:, n, :], in0=os_[:], scalar=s_t[:, 2:3], in1=G_[:, n, :], op0=ALU.mult, op1=ALU.mult),
                                          reads=[os_r, s_r, G_r], writes=[ya_r])
                                else:
                                    tm, tm_r = tmr.next()
                                    fw.op("dve", lambda: nc.vector.scalar_tensor_tensor(out=tm[:], in0=os_[:], scalar=s_t[:, 2:3], in1=G_[:, n, :], op0=ALU.mult, op1=ALU.mult),
                                          reads=[os_r, s_r, G_r], writes=[tm_r])
                                    fw.op("pool", lambda: nc.gpsimd.tensor_tensor(out=ya[:, n, :], in0=ya[:, n, :], in1=tm[:], op=ALU.add), reads=[tm_r, ya_r], writes=[ya_r])
                                if oi < 7:
                                    Kd, Kd_r = Kdr.next()
                                    fw.op("pool", lambda: nc.gpsimd.tensor_scalar(out=Kd[:], in0=Kh[:, n, :], scalar1=ex[:, k, 1:2], scalar2=None, op0=ALU.mult), reads=[Kh_r, r_ex], writes=[Kd_r])
                                    for dkc in range(2):
                                        q = k * 2 + dkc
                                        dS, dS_r = pS.next()
                                        fw.op("pe", lambda: nc.tensor.matmul(dS[:], lhsT=Kd[:, dkc * 128:(dkc + 1) * 128], rhs=Vh[:, n, :], start=True, stop=True), reads=[Kd_r, Vh_r], writes=[dS_r])
                                        fw.op("dve", lambda: nc.vector.scalar_tensor_tensor(out=Sf[:, q, :], in0=Sf[:, q, :], scalar=ex[:, k, 2:3], in1=dS[:], op0=ALU.mult, op1=ALU.add),
                                              reads=[dS_r, r_ex, r_S[q]], writes=[r_S[q]])
                                        fw.op("act", lambda: nc.scalar.copy(out=Sb_[:, q, :], in_=Sf[:, q, :]), reads=[r_S[q]], writes=[r_S[q]])
                        yb, yb_r = ybfr.next()
                        fw.op("act", lambda: nc.scalar.copy(out=yb[:], in_=ya[:]), reads=[ya_r], writes=[yb_r])
                        for c2 in range(2):
                            p_t, p_r = pT.next()
                            for n in range(8):
                                fw.op("pe", lambda: nc.tensor.transpose(out=p_t[:, n * 128:(n + 1) * 128], in_=yb[:, n, c2 * 128:(c2 + 1) * 128], identity=ident_bf[:]),
                                      reads=[yb_r, r_ident], writes=[p_r], signal=(n == 7))
                            yo, yo_r = yor.next()
                            fw.op("dve", lambda: nc.vector.tensor_copy(out=yo[:], in_=p_t[:]), reads=[p_r], writes=[yo_r])
                            fw.dma("sp", yretT[h * 2 + c2], yo[:], reads=[yo_r], writes=[rs("yretT")])
            fw.barrier()

        if "G" in PHASES:
            with contextlib.ExitStack() as st:
                mixT = sb(st, "mixT", [128, KC, NTOK], BF16); r_mix = Res()
                with contextlib.ExitStack() as st1:
                    ynT = sb(st1, "ynT", [128, 16, NTOK], BF16); yrT = sb(st1, "yrT", [128, 16, NTOK], BF16); r_yn = Res(); r_yr = Res()
                    fw.dma("sp", ynT[:], ynaT.rearrange("c p t -> p c t"), reads=[rs("ynaT")], writes=[r_yn])
                    fw.dma("sp", yrT[:], yretT.rearrange("c p t -> p c t"), reads=[rs("yretT")], writes=[r_yr])
                    WB = 128
                    wa = [sb(st1, "g_wa%d" % i, [128, 16, WB], BF16) for i in range(2)]; r_wa = [Res(), Res()]
                    wb = [sb(st1, "g_wb%d" % i, [128, 16, WB], BF16) for i in range(2)]; r_wb = [Res(), Res()]
                    gar = Rot([sb(st1, "g_ga%d" % i, [128, 512], F32) for i in range(2)])
                    gbr = Rot([sb(st1, "g_gb%d" % i, [128, 512], F32) for i in range(2)])
                    t1r = Rot([sb(st1, "g_t1%d" % i, [128, 512], F32) for i in range(2)])
                    t2r = Rot([sb(st1, "g_t2%d" % i, [128, 512], F32) for i in range(2)])
                    pp = Rot([ps(st1, "g_ps%d" % i, [128, 512]) for i in range(4)])
                    wa_v = w_bna.rearrange("(c p) n -> p c n", p=128); wb_v = w_bret.rearrange("(c p) n -> p c n", p=128)
                    nb = D // WB
                    fw.dma("pool", wa[0][:], wa_v[:, :, 0:WB], writes=[r_wa[0]]); fw.dma("pool", wb[0][:], wb_v[:, :, 0:WB], writes=[r_wb[0]])
                    for b in range(nb):
                        if b + 1 < nb:
                            s_ = (b + 1) % 2
                            fw.dma("pool", wa[s_][:], wa_v[:, :, (b + 1) * WB:(b + 2) * WB], writes=[r_wa[s_]])
                            fw.dma("pool", wb[s_][:], wb_v[:, :, (b + 1) * WB:(b + 2) * WB], writes=[r_wb[s_]])
                        ch = b
                        for t0 in (0, 512):
                            ga, ga_r = gar.next(); gb, gb_r = gbr.next()
                            fw.dma("sp", ga[:], GaT[ch, :, t0:t0 + 512], reads=[rs("GaT")], writes=[ga_r])
                            fw.dma("sp", gb[:], GbT[ch, :, t0:t0 + 512], reads=[rs("GbT")], writes=[gb_r])
                            pa, pa_r = pp.next()
                            mm_fm(pa, pa_r, wa[b % 2], r_wa[b % 2], 0, ynT, r_yn, t0, 512, kc=16)
                            pb, pb_r = pp.next()
                            mm_fm(pb, pb_r, wb[b % 2], r_wb[b % 2], 0, yrT, r_yr, t0, 512, kc=16)
                            t1, t1_r = t1r.next(); t2, t2_r = t2r.next()
                            fw.op("dve", lambda: nc.vector.tensor_tensor(out=t1[:], in0=pa[:], in1=ga[:], op=ALU.mult), reads=[pa_r, ga_r], writes=[t1_r])
                            fw.op("dve", lambda: nc.vector.tensor_tensor(out=t2[:], in0=pb[:], in1=gb[:], op=ALU.mult), reads=[pb_r, gb_r], writes=[t2_r])
                            fw.op("pool", lambda: nc.gpsimd.tensor_tensor(out=mixT[:, ch, t0:t0 + 512], in0=t1[:], in1=t2[:], op=ALU.add), reads=[t1_r, t2_r], writes=[r_mix])
                fw.barrier()
                with contextlib.ExitStack() as st1:
                    WB = 512
                    wt = [sb(st1, "go_wt%d" % i, [128, KC, WB], BF16) for i in range(2)]; r_wt = [Res(), Res()]
                    g1bc = sb(st1, "g1bc", [128, D], F32); r_g1 = Res()
                    mod_bc(g1bc, 2, 0, r_g1)
                    xor_ = Rot([sb(st1, "go_x%d" % i, [128, 512], F32) for i in range(3)])
                    tr_ = Rot([sb(st1, "go_t%d" % i, [128, 512], F32) for i in range(3)])
                    pp = Rot([ps(st1, "go_ps%d" % i, [128, 512]) for i in range(4)])
                    wo_v = w_out.rearrange("(c p) n -> p c n", p=128)

                    def go_compute(b, w_t, w_r):
                        for tt in range(8):
                            xo, xo_r = xor_.next()
                            fw.dma("sp", xo[:], x_own[tt * 128:(tt + 1) * 128, b * 512:(b + 1) * 512], writes=[xo_r])
                            p_t, p_r = pp.next()
                            mm_tm(p_t, p_r, w_t, w_r, 0, 512, mixT, r_mix, tt * 128)
                            t_, t_r = tr_.next()
                            fw.op("dve", lambda: nc.vector.tensor_tensor(out=t_[:], in0=p_t[:], in1=g1bc[:, b * 512:(b + 1) * 512], op=ALU.mult), reads=[p_r, r_g1], writes=[t_r])
                            fw.op("pool", lambda: nc.gpsimd.tensor_tensor(out=t_[:], in0=t_[:], in1=xo[:], op=ALU.add), reads=[xo_r, t_r], writes=[t_r])
                            fw.dma("sp", Xmid[tt * 128:(tt + 1) * 128, b * 512:(b + 1) * 512], t_[:], reads=[t_r], writes=[rs("Xmid")])

                    stream_weights(lambda b: wo_v[:, :, b * 512:(b + 1) * 512], 8, wt, r_wt, go_compute)
            fw.barrier()

        if "N2" in PHASES:
            with contextlib.ExitStack() as st:
                h2T = sb(st, "h2T", [128, KC, NTOK], BF16); r_h2T = Res()
                with contextlib.ExitStack() as st1:
                    tabA = sb(st1, "tabA3", [128, D], F32); tabB = sb(st1, "tabB3", [128, D], F32); r_tab = Res()
                    load_tabs(st1, tabA, tabB, r_tab, 0, 3, 4, norm2_row)
                    build_hT(st1, Xmid, NTOK, h2T, r_h2T, 0, tabA, tabB, r_tab, "n_", tm_out=h2_in, src_res=rs("Xmid"))
                fw.barrier()
                allgather_chunks(h2_in, h2_all, G4, 8, rs("tm_out"), rs("h2_all"))
                wr = sb(st, "wr", [128, KC, 16], BF16); r_wr = Res()
                fw.dma("pool", wr[:], w_router.rearrange("(c p) e -> p c e", p=128), writes=[r_wr])
                lpr = Rot([ps(st, "n2_lp%d" % i, [128, 16]) for i in range(2)])
                sm = Rot([sb(st, "n2_sm%d" % i, [128, 4], F32) for i in range(2)])
                et = Rot([sb(st, "n2_et%d" % i, [128, 16], F32) for i in range(2)])
                for tt in range(8):
                    lp, lp_r = lpr.next()
                    for c in range(KC):
                        fw.op("pe", lambda: nc.tensor.matmul(lp, lhsT=h2T[:, c, tt * 128:(tt + 1) * 128], rhs=wr[:, c, :], start=(c == 0), stop=(c == KC - 1)),
                              reads=[r_h2T, r_wr], writes=[lp_r], signal=(c == KC - 1))
                    s_t, s_r = sm.next(); e_t, e_r = et.next()
                    fw.op("dve", lambda: nc.vector.reduce_max(out=s_t[:, 0:1], in_=lp, axis=AX.X), reads=[lp_r], writes=[s_r])
                    fw.op("dve", lambda: nc.vector.tensor_scalar(out=s_t[:, 1:2], in0=s_t[:, 0:1], scalar1=-1.0, scalar2=None, op0=ALU.mult), reads=[s_r], writes=[s_r])
                    fw.op("act", lambda: nc.scalar.activation(out=e_t[:], in_=lp, func=AF.Exp, bias=s_t[:, 1:2], scale=1.0, accum_out=s_t[:, 2:3]), reads=[lp_r, s_r], writes=[e_r, s_r])
                    fw.op("dve", lambda: nc.vector.reciprocal(out=s_t[:, 3:4], in_=s_t[:, 2:3]), reads=[s_r], writes=[s_r])
                    fw.op("dve", lambda: nc.vector.tensor_scalar(out=aff_own[:, tt, :], in0=e_t[:], scalar1=s_t[:, 3:4], scalar2=None, op0=ALU.mult), reads=[e_r, s_r], writes=[r_aff])
                fw.dma("sp", aff_in.rearrange("(t p) e -> p t e", p=128), aff_own[:], reads=[r_aff], writes=[rs("aff_in")])
                allgather(aff_in, aff_all, G4, rs("aff_in"), rs("aff_all"))
            fw.barrier()

        if "E" in PHASES:
            with contextlib.ExitStack() as st:
                xeT = sb(st, "xeT", [128, KC, 512], BF16); r_xeT = Res()
                gateS = sb(st, "gateS", [128, 4, 4], F32); r_gate = Res()
                rk = sb(st, "rk", [128, 4, 32], F32); r_rk = Res()
                idx_i = sb(st, "idx_i", [128, 4, 4], I32); r_idx = Res()
                with contextlib.ExitStack() as st1:
                    A_sb = sb(st1, "A_sb", [128, 32, 16], F32); r_A = Res()
                    fw.dma("sp", A_sb[:], aff_all.rearrange("(g p) e -> p g e", p=128), reads=[rs("aff_all")], writes=[r_A])
                    ohEs = sb(st1, "ohEs", [128, 64], F32); r_oh = Res()
                    fw.dma("sp", ohEs[:], ohE[0, :].partition_broadcast(128), writes=[r_oh])
                    lm = sb(st1, "lm", [128, 128], F32); iot = sb(st1, "iot", [128, 512], F32); tok = sb(st1, "tok", [128, 32], F32)
                    fw.dma("sp", lm[:], lowmask, writes=[r_oh]); fw.dma("sp", iot[:], iota512, writes=[r_oh]); fw.dma("sp", tok[:], tokid, writes=[r_oh])
                    vcs = sb(st1, "vcs", [128, 4, 32], F32); r_vcs = Res()
                    fw.op("dve", lambda: nc.vector.memset(vcs[:], 0.0), writes=[r_vcs])
                    for el in range(4):
                        for e in range(16):
                            fw.op("dve", lambda: nc.vector.scalar_tensor_tensor(out=vcs[:, el, :], in0=A_sb[:, :, e], scalar=ohEs[:, el * 16 + e:el * 16 + e + 1], in1=vcs[:, el, :], op0=ALU.mult, op1=ALU.add),
                                  reads=[r_A, r_oh, r_vcs], writes=[r_vcs])
                    bkV = ps(st1, "e_bkV", [128, 512])
                    vt_sb = sb(st1, "vt_sb", [32, 4, 128], F32); r_vt = Res()
                    for el in range(4):
                        fw.op("pe", lambda: nc.tensor.transpose(out=bkV[0:32, el * 128:(el + 1) * 128], in_=vcs[:, el, :], identity=ident_f[:]), reads=[r_vcs, r_ident], writes=[r_vt])
                    fw.op("act", lambda: nc.scalar.copy(out=vt_sb[:], in_=bkV[0:32, 0:512].rearrange("p (e t) -> p e t", e=4)), reads=[r_vt], writes=[r_vt])
                    for el in range(4):
                        fw.dma("sp", vsel[el].rearrange("(g t) -> g t", t=128), vt_sb[:, el, :], reads=[r_vt], writes=[rs("vsel")])
                    vrr = Rot([sb(st1, "vrow%d" % i, [128, 4096], F32) for i in range(2)])
                    junk = sb(st1, "e_junk", [128, 4096], BF16); r_junk = Res()
                    r4 = sb(st1, "r4", [128, 32, 4], F32); r_r4 = Res()
                    TG = sb(st1, "TG", [128, 32, 2], F32); r_TG = Res()
                    OHr = Rot([sb(st1, "OH%d" % i, [128, 128], F32) for i in range(4)])
                    idxg = sb(st1, "idxg", [128, 4, 2], F32); r_ig = Res()
                    bkI = ps(st1, "e_bkI", [128, 512])
                    for el in range(4):
                        vr_, vr_r = vrr.next()
                        fw.dma("sp", vr_[:], vsel[el, :].partition_broadcast(128), reads=[rs("vsel")], writes=[vr_r])
                        fw.op("pool", lambda: nc.gpsimd.memset(r4[:], 0.0), writes=[r_r4])
                        for g in range(32):
                            vc = vcs[:, el, g:g + 1]
                            if g > 0:
                                fw.op("dve", lambda: nc.vector.tensor_scalar(out=junk[:, 0:g * 128], in0=vr_[:, 0:g * 128], scalar1=vc, scalar2=0.0, op0=ALU.is_ge, op1=ALU.add, accum_out=r4[:, g, 0:1]),
                                      reads=[vr_r, r_vcs], writes=[r_junk, r_r4])
                            if g < 31:
                                fw.op("dve", lambda: nc.vector.tensor_scalar(out=junk[:, (g + 1) * 128:4096], in0=vr_[:, (g + 1) * 128:4096], scalar1=vc, scalar2=0.0, op0=ALU.is_gt, op1=ALU.add, accum_out=r4[:, g, 1:2]),
                                      reads=[vr_r, r_vcs], writes=[r_junk, r_r4])
                            fw.op("dve", lambda: nc.vector.tensor_scalar(out=junk[:, g * 128:(g + 1) * 128], in0=vr_[:, g * 128:(g + 1) * 128], scalar1=vc, scalar2=0.0, op0=ALU.is_gt, op1=ALU.add, accum_out=r4[:, g, 2:3]),
                                  reads=[vr_r, r_vcs], writes=[r_junk, r_r4])
                            fw.op("dve", lambda: nc.vector.scalar_tensor_tensor(out=junk[:, g * 128:(g + 1) * 128], in0=vr_[:, g * 128:(g + 1) * 128], scalar=vc, in1=lm[:], op0=ALU.is_equal, op1=ALU.mult, accum_out=r4[:, g, 3:4]),
                                  reads=[vr_r, r_vcs, r_oh], writes=[r_junk, r_r4])
                        fw.op("dve", lambda: nc.vector.tensor_reduce(out=rk[:, el, :], in_=r4[:], axis=AX.X, op=ALU.add), reads=[r_r4], writes=[r_rk])
                        fw.op("pool", lambda: nc.gpsimd.tensor_copy(out=TG[:, :, 0], in_=tok[:, :]), reads=[r_oh], writes=[r_TG])
                        fw.op("pool", lambda: nc.gpsimd.tensor_copy(out=TG[:, :, 1], in_=vcs[:, el, :]), reads=[r_vcs], writes=[r_TG])
                        for s4 in range(4):
                            for g in range(32):
                                OH, OH_r = OHr.next()
                                fw.op("dve", lambda: nc.vector.tensor_scalar(out=OH[:], in0=iot[:, s4 * 128:(s4 + 1) * 128], scalar1=rk[:, el, g:g + 1], scalar2=None, op0=ALU.is_equal), reads=[r_rk, r_oh], writes=[OH_r])
                                fw.op("pe", lambda: nc.tensor.matmul(bkI[:, s4 * 2:(s4 + 1) * 2], lhsT=OH[:], rhs=TG[:, g, :], start=(g == 0), stop=(g == 31)),
                                      reads=[OH_r, r_TG], writes=[r_ig], signal=True)
                        fw.op("act", lambda: nc.scalar.copy(out=idxg[:], in_=bkI[:, 0:8].rearrange("p (s c) -> p s c", c=2)), reads=[r_ig], writes=[r_ig])
                        fw.op("dve", lambda: nc.vector.tensor_scalar(out=idxg[:, :, 0], in0=idxg[:, :, 0], scalar1=0.0, scalar2=4095.0, op0=ALU.max, op1=ALU.min), reads=[r_ig], writes=[r_ig])
                        fw.op("dve", lambda: nc.vector.tensor_copy(out=idx_i[:, el, :], in_=idxg[:, :, 0]), reads=[r_ig], writes=[r_idx])
                        fw.op("dve", lambda: nc.vector.tensor_copy(out=gateS[:, el, :], in_=idxg[:, :, 1]), reads=[r_ig], writes=[r_gate])
                    fw.dma("sp", rank_in, rk[:].rearrange("p a g -> p (a g)"), reads=[r_rk], writes=[rs("rank_in")])
                    allgather(rank_in, rank_all, G4, rs("rank_in"), rs("rank_all"))
                fw.barrier()
                for el in range(4):
                    with contextlib.ExitStack() as st1:
                        xer = Rot([sb(st1, "xe%d" % i, [128, D], BF16) for i in range(2)])
                        ptr = Rot([ps(st1, "e_ptr%d" % i, [128, 1024], BF16) for i in range(2)])
                        for b in range(1):
                            for s4 in range(4):
                                xe, xe_r = xer.next()
                                fw.custom("pool", lambda: nc.gpsimd.indirect_dma_start(
                                    out=xe[:], out_offset=None, in_=h2_all[:, :],
                                    in_offset=bass.IndirectOffsetOnAxis(ap=idx_i[:, el, s4:s4 + 1], axis=0)), 16,
                                    reads=[r_idx, rs("h2_all")], writes=[xe_r])
                                for g8 in range(4):
                                    p_t, p_r = ptr.next()
                                    for k in range(8):
                                        c = g8 * 8 + k
                                        fw.op("pe", lambda: nc.tensor.transpose(out=p_t[:, k * 128:(k + 1) * 128], in_=xe[:, c * 128:(c + 1) * 128], identity=ident_bf[:]),
                                              reads=[xe_r, r_ident], writes=[p_r], signal=(k == 7))
                                    dst = xeT[:, g8 * 8:(g8 + 1) * 8, b * 512 + s4 * 128:b * 512 + (s4 + 1) * 128]
                                    src = p_t[:].rearrange("p (k t) -> p k t", k=8)
                                    if g8 % 2 == 0:
                                        fw.op("act", lambda: nc.scalar.copy(out=dst, in_=src), reads=[p_r], writes=[r_xeT])
                                    else:
                                        fw.op("dve", lambda: nc.vector.tensor_copy(out=dst, in_=src), reads=[p_r], writes=[r_xeT])
                    fw.barrier()
                    with contextlib.ExitStack() as st2:
                        hT_ = sb(st2, "e_hT", [128, 16, 512], BF16); r_hT_ = Res()
                        pp = Rot([ps(st2, "e_ps%d" % i, [128, 512]) for i in range(4)])
                        with contextlib.ExitStack() as st3:
                            WB = 256
                            wg = [sb(st3, "e_wg%d" % i, [128, KC, WB], BF16) for i in range(2)]; r_wg = [Res(), Res()]
                            wu = [sb(st3, "e_wu%d" % i, [128, KC, WB], BF16) for i in range(2)]; r_wu = [Res(), Res()]
                            sar = Rot([sb(st3, "e_sa%d" % i, [128, 512], F32) for i in range(2)])
                            wg_v = w_gate_s[el].rearrange("(c p) f -> p c f", p=128); wu_v = w_up_s[el].rearrange("(c p) f -> p c f", p=128)
                            nb = 2048 // WB
                            fw.dma("pool", wg[0][:], wg_v[:, :, 0:WB], writes=[r_wg[0]]); fw.dma("pool", wu[0][:], wu_v[:, :, 0:WB], writes=[r_wu[0]])
                            for bb in range(nb):
                                if bb + 1 < nb:
                                    s_ = (bb + 1) % 2
                                    fw.dma("pool", wg[s_][:], wg_v[:, :, (bb + 1) * WB:(bb + 2) * WB], writes=[r_wg[s_]])
                                    fw.dma("pool", wu[s_][:], wu_v[:, :, (bb + 1) * WB:(bb + 2) * WB], writes=[r_wu[s_]])
                                for sub in range(WB // 128):
                                    ft = bb * (WB // 128) + sub
                                    for t0 in (0,):
                                        pa, pa_r = pp.next()
                                        mm_fm(pa, pa_r, wg[bb % 2], r_wg[bb % 2], sub * 128, xeT, r_xeT, t0, 512)
                                        pu, pu_r = pp.next()
                                        mm_fm(pu, pu_r, wu[bb % 2], r_wu[bb % 2], sub * 128, xeT, r_xeT, t0, 512)
                                        sa, sa_r = sar.next()
                                        fw.op("act", lambda: nc.scalar.activation(out=sa[:], in_=pa[:], func=AF.Silu), reads=[pa_r], writes=[sa_r])
                                        fw.op("dve", lambda: nc.vector.tensor_tensor(out=hT_[:, ft, t0:t0 + 512], in0=pu[:], in1=sa[:], op=ALU.mult), reads=[pu_r, sa_r], writes=[r_hT_])
                        fw.barrier()
                        with contextlib.ExitStack() as st3:
                            wd = [sb(st3, "e_wd%d" % i, [128, 16, 512], BF16) for i in range(2)]; r_wd = [Res(), Res()]
                            yor = Rot([sb(st3, "e_yo%d" % i, [128, 512], BF16) for i in range(3)])
                            wd_v = w_down_s[el].rearrange("(c p) n -> p c n", p=128)

                            def dn_compute(cb, w_t, w_r):
                                for s8 in range(4):
                                    p_t, p_r = pp.next()
                                    mm_tm(p_t, p_r, w_t, w_r, 0, 512, hT_, r_hT_, s8 * 128, kc=16)
                                    yo, yo_r = yor.next()
                                    fw.op("dve", lambda: nc.vector.tensor_scalar(out=yo[:], in0=p_t[:], scalar1=gateS[:, el, s8:s8 + 1], scalar2=None, op0=ALU.mult), reads=[p_r, r_gate], writes=[yo_r])
                                    r0 = el * 512 + s8 * 128
                                    fw.dma("sp", ye_in[r0:r0 + 128, cb * 512:(cb + 1) * 512], yo[:], reads=[yo_r], writes=[rs("ye_in")])

                            stream_weights(lambda cb: wd_v[:, :, cb * 512:(cb + 1) * 512], 8, wd, r_wd, dn_compute)
                    fw.barrier()
                allgather_chunks(ye_in, ye_all, G4, 16, rs("ye_in"), rs("ye_all"))
            fw.barrier()

        if "C" in PHASES:
            with contextlib.ExitStack() as st:
                Rall = sb(st, "Rall", [128, 4, 128], F32); r_Rall = Res()
                fw.dma("sp", Rall[:], rank_all.rearrange("(c p) f -> p c f", p=128), reads=[rs("rank_all")], writes=[r_Rall])
                ohIs = sb(st, "ohIs", [128, 4], F32); ebs = sb(st, "ebs", [128, 16], F32); r_c0 = Res()
                fw.dma("sp", ohIs[:], ohI[0, :].partition_broadcast(128), writes=[r_c0]); fw.dma("sp", ebs[:], ebase, writes=[r_c0])
                mine = sb(st, "mine", [128, 16, 8], F32); r_mine = Res()
                fw.op("dve", lambda: nc.vector.memset(mine[:], 0.0), writes=[r_mine])
                Rv = Rall[:].rearrange("p c (a g) -> p c a g", a=4)
                for i in range(4):
                    for c in range(4):
                        fw.op("dve", lambda: nc.vector.scalar_tensor_tensor(out=mine[:, 4 * c:4 * c + 4, :], in0=Rv[:, c, :, i * 8:(i + 1) * 8], scalar=ohIs[:, i:i + 1], in1=mine[:, 4 * c:4 * c + 4, :], op0=ALU.mult, op1=ALU.add),
                              reads=[r_Rall, r_c0, r_mine], writes=[r_mine])
                selm = sb(st, "selm", [128, 16, 8], F32); idf = sb(st, "idf", [128, 16, 8], F32); idi = sb(st, "idi", [128, 16, 8], I32)
                fw.op("dve", lambda: nc.vector.tensor_scalar(out=selm[:], in0=mine[:], scalar1=512.0, scalar2=None, op0=ALU.is_lt), reads=[r_mine], writes=[r_mine])
                fw.op("dve", lambda: nc.vector.tensor_scalar(out=idf[:], in0=mine[:], scalar1=511.0, scalar2=0.0, op0=ALU.min, op1=ALU.max), reads=[r_mine], writes=[r_mine])
                qf = sb(st, "qf", [128, 16, 8], F32)
                fw.op("dve", lambda: nc.vector.memset(qf[:], 0.0), writes=[r_mine])
                for kq in range(1, 4):
                    fw.op("dve", lambda: nc.vector.scalar_tensor_tensor(out=qf[:].rearrange("p e g -> p (e g)"), in0=idf[:].rearrange("p e g -> p (e g)"), scalar=128.0 * kq, in1=qf[:].rearrange("p e g -> p (e g)"), op0=ALU.is_ge, op1=ALU.add), reads=[r_mine], writes=[r_mine])
                fw.op("dve", lambda: nc.vector.scalar_tensor_tensor(out=idf[:].rearrange("p e g -> p (e g)"), in0=qf[:].rearrange("p e g -> p (e g)"), scalar=384.0, in1=idf[:].rearrange("p e g -> p (e g)"), op0=ALU.mult, op1=ALU.add), reads=[r_mine], writes=[r_mine])
                for e in range(16):
                    fw.op("dve", lambda: nc.vector.tensor_scalar(out=idf[:, e, :], in0=idf[:, e, :], scalar1=ebs[:, e:e + 1], scalar2=None, op0=ALU.add), reads=[r_mine, r_c0], writes=[r_mine])
                fw.op("dve", lambda: nc.vector.tensor_copy(out=idi[:], in_=idf[:]), reads=[r_mine], writes=[r_mine])
                g2bc = sb(st, "g2bc", [128, D], F32); fnbc = sb(st, "fnbc", [128, D], F32); r_g2 = Res()
                mod_bc(g2bc, 5, 0, r_g2)
                fw.dma("sp", fnbc[:], fnorm_row[0, :].partition_broadcast(128), writes=[r_g2])
                accr = Rot([sb(st, "c_acc%d" % i, [128, D], F32) for i in range(2)])
                xmr = Rot([sb(st, "c_xm%d" % i, [128, D], F32) for i in range(2)])
                gar = Rot([sb(st, "c_ga%d" % i, [128, D], BF16) for i in range(3)])
                cjunk = sb(st, "c_junk", [128, D], BF16); r_cj = Res()
                cst = Rot([sb(st, "c_st%d" % i, [128, 4], F32) for i in range(2)])
                for g in range(8):
                    acc, acc_r = accr.next(); xm, xm_r = xmr.next()
                    fw.dma("sp", xm[:], Xmid[g * 128:(g + 1) * 128, :], reads=[rs("Xmid")], writes=[xm_r])
                    for e in range(16):
                        ga, ga_r = gar.next()
                        fw.custom("pool", lambda: nc.gpsimd.indirect_dma_start(
                            out=ga[:], out_offset=None, in_=ye_all[:, :],
                            in_offset=bass.IndirectOffsetOnAxis(ap=idi[:, e, g:g + 1], axis=0)), 16,
                            reads=[r_mine, rs("ye_all")], writes=[ga_r])
                        if e == 0:
                            fw.op("dve", lambda: nc.vector.tensor_scalar(out=acc[:], in0=ga[:], scalar1=selm[:, e, g:g + 1], scalar2=None, op0=ALU.mult), reads=[ga_r, r_mine], writes=[acc_r])
                        else:
                            fw.op("dve", lambda: nc.vector.scalar_tensor_tensor(out=acc[:], in0=ga[:], scalar=selm[:, e, g:g + 1], in1=acc[:], op0=ALU.mult, op1=ALU.add), reads=[ga_r, r_mine, acc_r], writes=[acc_r])
                    fw.op("dve", lambda: nc.vector.tensor_tensor(out=acc[:], in0=acc[:], in1=g2bc[:], op=ALU.mult), reads=[acc_r, r_g2], writes=[acc_r])
                    fw.op("pool", lambda: nc.gpsimd.tensor_tensor(out=acc[:], in0=acc[:], in1=xm[:], op=ALU.add), reads=[acc_r, xm_r], writes=[acc_r])
                    s_t, s_r = cst.next()
                    fw.op("act", lambda: nc.scalar.activation(out=cjunk[:], in_=acc[:], func=AF.Square, accum_out=s_t[:, 0:1]), reads=[acc_r], writes=[r_cj, s_r])
                    fw.op("act", lambda: nc.scalar.activation(out=s_t[:, 1:2], in_=s_t[:, 0:1], func=AF.Ln, scale=1.0 / D, bias=EPS), reads=[s_r], writes=[s_r])
                    fw.op("act", lambda: nc.scalar.activation(out=s_t[:, 2:3], in_=s_t[:, 1:2], func=AF.Exp, scale=-0.5), reads=[s_r], writes=[s_r])
                    fw.op("dve", lambda: nc.vector.scalar_tensor_tensor(out=xm[:], in0=acc[:], scalar=s_t[:, 2:3], in1=fnbc[:], op0=ALU.mult, op1=ALU.mult), reads=[acc_r, s_r, r_g2, xm_r], writes=[xm_r])
                    fw.dma("sp", out[g * 128:(g + 1) * 128, :], xm[:], reads=[xm_r], writes=[rs("out")])
            fw.barrier()
        fw.barrier()
    return nc


def _host_inputs(inp):
    x = np.asarray(inp["x"], np.float32); c = np.asarray(inp["c"], np.float32)
    ctx = np.asarray(inp["ctx"], np.float32); c_ctx = np.asarray(inp["c_ctx"], np.float32)
    w_mod = np.asarray(inp["w_mod"], np.float32)[0]; b_mod = np.asarray(inp["b_mod"], np.float32)[0]
    w_in = np.ascontiguousarray(np.asarray(inp["w_in"], np.float32)[0])
    rpb = np.asarray(inp["na_rpb"], np.float32)[0]
    common = {}
    common["w_in"] = w_in
    common["norm1_row"] = np.asarray(inp["norm1"], np.float32).reshape(1, D)
    common["norm2_row"] = np.asarray(inp["norm2"], np.float32).reshape(1, D)
    common["fnorm_row"] = np.asarray(inp["final_norm"], np.float32).reshape(1, D)
    common["ret_decay"] = np.asarray(inp["ret_decay"], np.float32).reshape(1, 16)
    common["w_bna"] = np.ascontiguousarray(np.asarray(inp["w_branch_na"], np.float32)[0])
    common["w_bret"] = np.ascontiguousarray(np.asarray(inp["w_branch_ret"], np.float32)[0])
    common["w_out"] = np.ascontiguousarray(np.asarray(inp["w_out"], np.float32)[0])
    common["w_router"] = np.ascontiguousarray(np.asarray(inp["w_router"], np.float32)[0])
    common["ident_in"] = np.eye(128, dtype=np.float32)
    common["iota512"] = np.tile(np.arange(512, dtype=np.float32)[None], (128, 1))
    pp_ = np.arange(128)
    common["lowmask"] = (pp_[None, :] < pp_[:, None]).astype(np.float32)
    kc = np.arange(64); qc = np.arange(64)
    c0 = np.clip(qc - 8, 0, 48)
    col_ok = (kc[:, None] >= c0[None, :]) & (kc[:, None] < c0[None, :] + 16)
    dc = np.clip(kc[:, None] - qc[None, :], -15, 15) + 15
    nab = np.full((16, 2, 64, 7, 2, 64), -1e30, np.float32)
    for di in range(7):
        delta = 2 * di - 2
        for sr in range(2):
            for rr in range(2):
                dr = delta + sr - rr + 3
                if 0 <= dr <= 14:
                    blk = rpb[:, dr][:, dc]
                    nab[:, sr, :, di, rr, :] = np.where(col_ok[None], blk, np.float32(-1e30))
    common["nabias"] = nab.reshape(16, 128, 7 * 128)
    oh = np.zeros((128, 24, 64), np.float32)
    for s in range(24):
        oh[s, s, :] = 1.0
    common["onehotA"] = oh.reshape(128, NKV)
    p = np.arange(128, dtype=np.float32)
    Mf = np.where(p[None, :] >= p[:, None], p[None, :] - p[:, None], 1e9).astype(np.float32)
    Mb = np.where(p[:, None] >= p[None, :], p[:, None] - p[None, :], 1e9).astype(np.float32)
    common["retM"] = np.concatenate([Mf, Mb], axis=1)
    rp = np.zeros((128, 26), np.float32)
    rp[:, 0] = p + 1; rp[:, 1] = 127 - p; rp[:, 2] = 128.0
    rp[:, 13] = 128 - p; rp[:, 14] = p; rp[:, 15] = 128.0
    for t in range(8):
        rp[:, 3 + t] = 1023 - (t * 128 + p)
        rp[:, 16 + t] = t * 128 + p
    rp[:, 11] = 255 - p; rp[:, 12] = 127 - p
    rp[:, 24] = p; rp[:, 25] = 128 + p
    common["retpos"] = rp
    inv = (10000.0 ** (-np.arange(64, dtype=np.float32) / 64)).astype(np.float32)
    per_core = []
    wg = np.asarray(inp["w_gate"], np.float32)[0]; wu = np.asarray(inp["w_up"], np.float32)[0]; wd = np.asarray(inp["w_down"], np.float32)[0]
    for j in range(8):
        b = j // 4; jj = j % 4
        d = dict(common)
        d["x_own"] = np.ascontiguousarray(x[b, jj * 1024:(jj + 1) * 1024])
        xkv = np.zeros((24, 64, D), np.float32)
        for s in range(24):
            KR = 16 * jj - 4 + s
            if 0 <= KR < 64:
                xkv[s] = x[b, KR * 64:(KR + 1) * 64]
        d["x_kv"] = xkv.reshape(NKV, D)
        d["ctxb"] = np.ascontiguousarray(ctx[b])
        cc = np.stack([c[b], c_ctx], axis=-1)
        d["cT"] = np.ascontiguousarray(cc.reshape(KC, 128, 2).transpose(1, 0, 2))
        d["w_mod_s"] = np.ascontiguousarray(w_mod[:, jj * 6144:(jj + 1) * 6144])
        d["bmod_row"] = np.ascontiguousarray(b_mod[jj * 6144:(jj + 1) * 6144]).reshape(1, 6144)
        t = np.arange(NTOK)
        Rw = (16 * jj + t // 64).astype(np.float32); Cl = (t % 64).astype(np.float32)
        ang_r = Rw[:, None] * inv[None, :]; ang_c = Cl[:, None] * inv[None, :]
        cs = np.concatenate([np.cos(ang_r), np.cos(ang_c)], axis=1).astype(np.float32)
        sn = np.concatenate([np.sin(ang_r), np.sin(ang_c)], axis=1).astype(np.float32)
        d["ropeC"] = cs; d["ropeS"] = sn
        d["ropeCk"] = (cs / 16.0).astype(np.float32); d["ropeSk"] = (sn / 16.0).astype(np.float32)
        vb = np.zeros((128, 16, 64), np.float32)
        for r in range(16):
            Rg = 16 * jj + r
            r0 = min(max(Rg - 4, 0), 56)
            for s in range(24):
                KR = 16 * jj - 4 + s
                if not (r0 <= KR < r0 + 8):
                    vb[s, r, :] = -30000.0
        d["validB"] = vb.reshape(128, NTOK)
        E = np.zeros((1, 10), np.float32); M = np.zeros((1, 10), np.float32)
        for i in range(4):
            if i < jj:
                E[0, i] = 1024.0 * (jj - 1 - i); M[0, i] = 1.0
            if i > jj:
                E[0, 5 + i] = 1024.0 * (i - jj - 1); M[0, 5 + i] = 1.0
        E[0, 4] = 1024.0 * jj; M[0, 4] = 1.0
        E[0, 9] = 1024.0 * (3 - jj); M[0, 9] = 1.0
        d["ccE"] = E; d["ccM"] = M
        d["w_gate_s"] = np.ascontiguousarray(wg[4 * jj:4 * jj + 4]); d["w_up_s"] = np.ascontiguousarray(wu[4 * jj:4 * jj + 4])
        d["w_down_s"] = np.ascontiguousarray(wd[4 * jj:4 * jj + 4])
        oe = np.zeros((1, 64), np.float32)
        for el in range(4):
            oe[0, el * 16 + 4 * jj + el] = 1.0
        d["ohE"] = oe
        oi = np.zeros((1, 4), np.float32); oi[0, jj] = 1.0
        d["ohI"] = oi
        g = np.arange(32)
        ntok_ = g[None, :] * 128 + np.arange(128)[:, None]
        ci = ntok_ // 1024; tl = ntok_ % 1024
        d["tokid"] = ((tl // 128) * 512 + ci * 128 + (tl % 128)).astype(np.float32)
        eb = np.zeros((128, 16), np.float32)
        for e in range(16):
            eb[:, e] = ((e % 4) * 4) * 512 + (e // 4) * 128
        d["ebase"] = eb
        per_core.append(d)
    return per_core


_NC = None


def kernel(**inputs):
    global _NC
    per_core = _host_inputs(inputs)
    if _NC is None:
        _NC = build_nc()
    res = run_bass_kernel_spmd(_NC, per_core, core_ids=list(range(8)))
    outs = [np.asarray(r["out"], np.float32) for r in res.results]
    full = np.zeros((2, 4096, D), np.float32)
    for j in range(8):
        full[j // 4, (j % 4) * 1024:(j % 4 + 1) * 1024] = outs[j]
    return full
```

```python
import contextlib
import numpy as np
import concourse.bass as bass
import concourse.mybir as mybir
from concourse.bass_utils import run_bass_kernel_spmd

F32 = mybir.dt.float32
BF16 = mybir.dt.bfloat16
I32 = mybir.dt.int32
AF = mybir.ActivationFunctionType
ALU = mybir.AluOpType
AX = mybir.AxisListType

D = 4096
KC = 32
NTOK = 1024
NKV = 1536
NCTX = 256
EPS = 1e-6
DIN = 24576
DBG = ()
PHASES = ("P1", "P2", "NA", "R", "G", "N2", "E", "C")
XMID_IN = False
SKIP_IN = ()


class Res:
    __slots__ = ("name", "w", "r", "multi", "ws")

    def __init__(self, name="", multi=False):
        self.name = name
        self.w = None
        self.r = []
        self.multi = multi
        self.ws = {}


class FW:
    EPOCH = 3000

    def __init__(self, nc, stack, n_dma_sems=64):
        self.nc = nc
        self.stack = stack
        self.E = {"pe": nc.tensor, "act": nc.scalar, "dve": nc.vector, "pool": nc.gpsimd, "sp": nc.sync}
        self.sem = {}
        self.cnt = {}
        self.ep = {}
        self.final = {}
        for e in ("pe", "act", "dve", "pool"):
            self.ep[e] = 0
            self.sem[(e, 0)] = stack.enter_context(nc.semaphore("s_%s_0" % e))
            self.cnt[e] = 0
        self.dsem = [stack.enter_context(nc.semaphore("d%d" % i)) for i in range(n_dma_sems)]
        self.dcnt = [0] * n_dma_sems
        self.dnext = 0
        self.seen = {e: {} for e in self.E}
        self.log = {e: [] for e in self.E}

    def _wait(self, eng, tok):
        if tok is None:
            return
        kind, key, n = tok
        if kind == "e":
            if key[0] == eng and eng == "pe":
                return
            sem = self.sem[key]
        else:
            sem = self.dsem[key]
        k = (kind, key)
        if self.seen[eng].get(k, 0) >= n:
            return
        self.E[eng].wait_ge(sem, n)
        self.log[eng].append(("w", k, n))
        self.seen[eng][k] = n

    def deps(self, eng, reads, writes):
        for r in reads:
            if r.multi:
                for k, n in r.ws.items():
                    self._wait(eng, (k[0], k[1], n))
            else:
                self._wait(eng, r.w)
        for w in writes:
            if not w.multi:
                self._wait(eng, w.w)
            for t in w.r:
                self._wait(eng, t)

    def _mark(self, tok, reads, writes):
        for r in reads:
            r.r.append(tok)
            if len(r.r) > 64:
                r.r = r.r[-48:]
        for w in writes:
            if w.multi:
                k = (tok[0], tok[1])
                if w.ws.get(k, 0) < tok[2]:
                    w.ws[k] = tok[2]
                if w.r:
                    w.r = []
            else:
                w.w = tok
                w.r = []

    def op(self, eng, ins_fn, reads=(), writes=(), signal=True):
        self.deps(eng, reads, writes)
        ins = ins_fn()
        if signal:
            self.cnt[eng] += 1
            ins.then_inc(self.sem[(eng, self.ep[eng])], 1)
            tok = ("e", (eng, self.ep[eng]), self.cnt[eng])
            self.log[eng].append(("i", ("e", (eng, self.ep[eng])), 1))
            if self.cnt[eng] >= self.EPOCH:
                self.ep[eng] += 1
                self.cnt[eng] = 0
                self.sem[(eng, self.ep[eng])] = self.stack.enter_context(self.nc.semaphore("s_%s_%d" % (eng, self.ep[eng])))
        else:
            tok = ("e", (eng, self.ep[eng]), self.cnt[eng] + 1)
        self._mark(tok, reads, writes)
        return ins

    def _dpre(self, q):
        i = self.dnext
        if self.dcnt[i] > 0:
            self._wait(q, ("d", i, self.dcnt[i]))

    def _dtok(self, ins, inc):
        i = self.dnext
        self.dnext = (self.dnext + 1) % len(self.dsem)
        self.dcnt[i] += inc
        ins.then_inc(self.dsem[i], inc)
        self.log[self._curq].append(("i", ("d", i), inc))
        return ("d", i, self.dcnt[i])

    def dma(self, q, out, in_, reads=(), writes=(), **kw):
        self._curq = q
        self.deps(q, reads, writes)
        self._dpre(q)
        ins = self.E[q].dma_start(out=out, in_=in_, **kw)
        tok = self._dtok(ins, 16)
        self._mark(tok, reads, writes)
        return tok

    def custom(self, q, fn, inc, reads=(), writes=()):
        self._curq = q
        self.deps(q, reads, writes)
        self._dpre(q)
        ins = fn()
        tok = self._dtok(ins, inc)
        self._mark(tok, reads, writes)
        return tok

    def barrier(self):
        for e in self.E:
            for e2 in self.cnt:
                if self.cnt[e2] > 0:
                    self._wait(e, ("e", (e2, self.ep[e2]), self.cnt[e2]))
                elif self.ep[e2] > 0:
                    self._wait(e, ("e", (e2, self.ep[e2] - 1), self.EPOCH))
            for i, c in enumerate(self.dcnt):
                if c > 0:
                    self._wait(e, ("d", i, c))


class Rot:
    def __init__(self, tiles):
        self.t = tiles
        self.r = [Res() for _ in tiles]
        self.i = 0

    def next(self):
        k = self.i % len(self.t)
        self.i += 1
        return self.t[k], self.r[k]


def build_nc():
    nc = bass.Bass("TRN2", target_bir_lowering=False)

    def din(name, shape, dt=F32):
        if name in SKIP_IN:
            return None
        return nc.dram_tensor(name, list(shape), dt, kind="ExternalInput").ap()

    def dscr(name, shape, dt):
        kind = "ExternalOutput" if name in DBG else "Internal"
        return nc.dram_tensor(name, list(shape), dt, kind=kind).ap()

    x_own = din("x_own", [NTOK, D]); x_kv = din("x_kv", [NKV, D]); ctxb = din("ctxb", [NCTX, D])
    cT = din("cT", [128, KC, 2])
    w_mod_s = din("w_mod_s", [D, 6144]); bmod_row = din("bmod_row", [1, 6144])
    norm1_row = din("norm1_row", [1, D]); norm2_row = din("norm2_row", [1, D]); fnorm_row = din("fnorm_row", [1, D])
    w_in = din("w_in", [D, DIN])
    ropeC = din("ropeC", [NTOK, 128]); ropeS = din("ropeS", [NTOK, 128])
    ropeCk = din("ropeCk", [NTOK, 128]); ropeSk = din("ropeSk", [NTOK, 128])
    nabias = din("nabias", [16, 128, 7 * 128])
    validB = din("validB", [128, NTOK]); onehotA = din("onehotA", [128, NKV])
    ret_decay = din("ret_decay", [1, 16])
    retM = din("retM", [128, 256])
    retpos = din("retpos", [128, 26])
    ccE = din("ccE", [1, 10]); ccM = din("ccM", [1, 10])
    w_bna = din("w_bna", [2048, D]); w_bret = din("w_bret", [2048, D]); w_out = din("w_out", [D, D])
    w_router = din("w_router", [D, 16])
    w_gate_s = din("w_gate_s", [4, D, 2048]); w_up_s = din("w_up_s", [4, D, 2048]); w_down_s = din("w_down_s", [4, 2048, D])
    ident_in = din("ident_in", [128, 128])
    ohE = din("ohE", [1, 64]); ohI = din("ohI", [1, 4]); lowmask = din("lowmask", [128, 128])
    iota512 = din("iota512", [128, 512]); tokid = din("tokid", [128, 32])
    ebase = din("ebase", [128, 16])
    out = nc.dram_tensor("out", [NTOK, D], F32, kind="ExternalOutput").ap()

    modg_in = dscr("modg_in", [2, 6144], F32); modg_out = dscr("modg_out", [8, 6144], F32)
    QaT = dscr("QaT", [16, 128, NTOK], BF16); KaT = dscr("KaT", [16, 128, NKV + NCTX], BF16)
    Va = dscr("Va", [NKV + NCTX, 2048], BF16)
    QrT = dscr("QrT", [8, 128, 2, NTOK], BF16); KrT = dscr("KrT", [8, 128, 2, NTOK], BF16)
    Kr = dscr("Kr", [NTOK, 2048], BF16); Vr = dscr("Vr", [NTOK, 2048], BF16)
    Krc = dscr("Krc", [NCTX, 2048], BF16); Vrc = dscr("Vrc", [NCTX, 2048], BF16)
    Grf = dscr("Grf", [NTOK, 2048], F32); Grb = dscr("Grb", [NTOK, 2048], F32)
    GaT = dscr("GaT", [32, 128, NTOK], F32); GbT = dscr("GbT", [32, 128, NTOK], F32)
    Lst_in = dscr("Lst_in", [16 * 2 * 128, 256], F32); Lst_out = dscr("Lst_out", [4 * 16 * 2 * 128, 256], F32)
    S0d = dscr("S0d", [16 * 2 * 128, 256], F32)
    ynaT = dscr("ynaT", [16, 128, NTOK], BF16); yretT = dscr("yretT", [16, 128, NTOK], BF16)
    Xmid = din("Xmid", [NTOK, D]) if XMID_IN else dscr("Xmid", [NTOK, D], F32)
    h2_in = dscr("h2_in", [NTOK, D], BF16); h2_all = dscr("h2_all", [4 * NTOK, D], BF16)
    aff_in = dscr("aff_in", [NTOK, 16], F32); aff_all = dscr("aff_all", [4 * NTOK, 16], F32)
    vsel = dscr("vsel", [4, 4 * NTOK], F32)
    rank_in = dscr("rank_in", [128, 128], F32); rank_all = dscr("rank_all", [4 * 128, 128], F32)
    ye_in = dscr("ye_in", [2048, D], BF16); ye_all = dscr("ye_all", [4 * 2048, D], BF16)

    G8 = [list(range(8))]
    G4 = [[0, 1, 2, 3], [4, 5, 6, 7]]

    with contextlib.ExitStack() as top:
        fw = FW(nc, top)
        nc._fw = fw
        R_scr = {}

        def rs(name):
            if name not in R_scr:
                R_scr[name] = Res(name, multi=True)
            return R_scr[name]

        uid = [0]

        def sb(st, name, shape, dt):
            uid[0] += 1
            return st.enter_context(nc.sbuf_tensor("%s_%d" % (name, uid[0]), list(shape), dt))

        def ps(st, name, shape, dt=F32):
            uid[0] += 1
            esz = 4 if dt == F32 else 2
            per_bank = 2048 // esz
            free = 1
            for d_ in shape[1:]:
                free *= d_
            nbank = (free + per_bank - 1) // per_bank
            t = st.enter_context(nc.psum_tensor("%s_%d" % (name, uid[0]), [128, nbank * per_bank], dt))
            return t[0:shape[0], 0:free]

        def allgather(src, dst, groups, rsrc, rdst):
            fw.custom("pool", lambda: nc.gpsimd.collective_compute(
                "AllGather", ALU.bypass, replica_groups=groups, ins=[src.opt()], outs=[dst.opt()]),
                1, reads=[rsrc], writes=[rdst])

        def allgather_chunks(src, dst, groups, nchunks, rsrc, rdst):
            nr = len(groups[0])
            rc = src.shape[0] // nchunks
            for c in range(nchunks):
                s_ap = src[c * rc:(c + 1) * rc, :]; d_ap = dst[c * nr * rc:(c + 1) * nr * rc, :]
                if src.dtype == BF16:
                    s_ap = s_ap.bitcast(F32); d_ap = d_ap.bitcast(F32)
                allgather(s_ap, d_ap, groups, rsrc, rdst)

        ident_bf = sb(top, "ident_bf", [128, 128], BF16); ident_f = sb(top, "ident_f", [128, 128], F32)
        r_ident = Res("ident")
        fw.dma("sp", ident_f[:], ident_in, writes=[r_ident])
        fw.op("dve", lambda: nc.vector.tensor_copy(out=ident_bf[:], in_=ident_f[:]), reads=[r_ident], writes=[r_ident])

        def mod_bc(dst, seg, row, r_dst, q="sp"):
            n0 = seg * 4096
            pos = 0
            while pos < 4096:
                n = n0 + pos
                i = n // 6144
                lo = n % 6144
                ln = min(4096 - pos, 6144 - lo)
                fw.dma(q, dst[:, pos:pos + ln], modg_out[i * 2 + row, lo:lo + ln].partition_broadcast(128),
                       reads=[rs("modg_out")], writes=[r_dst])
                pos += ln

        with contextlib.ExitStack() as st:
            cTf = sb(st, "cTf", [128, KC, 2], F32); cTb = sb(st, "cTb", [128, KC, 2], BF16); r_c = Res()
            wt = [sb(st, "m_wt%d" % i, [128, KC, 512], BF16) for i in range(2)]; r_wt = [Res(), Res()]
            bm = sb(st, "bm", [2, 6144], F32); r_bm = Res()
            mrow = sb(st, "mrow", [2, 6144], F32); r_mrow = Res()
            pm = Rot([ps(st, "pm%d" % i, [2, 512]) for i in range(2)])
            fw.dma("sp", cTf[:], cT, writes=[r_c])
            fw.dma("sp", bm[:], bmod_row[0, :].partition_broadcast(2), writes=[r_bm])
            fw.op("act", lambda: nc.scalar.activation(out=cTb[:], in_=cTf[:], func=AF.Silu), reads=[r_c], writes=[r_c])
            wv = w_mod_s.rearrange("(c p) n -> p c n", p=128)
            fw.dma("pool", wt[0][:], wv[:, :, 0:512], writes=[r_wt[0]])
            for b in range(12):
                if b + 1 < 12:
                    fw.dma("pool", wt[(b + 1) % 2][:], wv[:, :, (b + 1) * 512:(b + 2) * 512], writes=[r_wt[(b + 1) % 2]])
                p_t, p_r = pm.next()
                for c in range(KC):
                    fw.op("pe", lambda: nc.tensor.matmul(p_t[:], lhsT=cTb[:, c, :], rhs=wt[b % 2][:, c, :], start=(c == 0), stop=(c == KC - 1)),
                          reads=[r_c, r_wt[b % 2]], writes=[p_r], signal=(c == KC - 1))
                fw.op("dve", lambda: nc.vector.tensor_tensor(out=mrow[:, b * 512:(b + 1) * 512], in0=p_t[:], in1=bm[:, b * 512:(b + 1) * 512], op=ALU.add),
                      reads=[p_r, r_bm], writes=[r_mrow])
            fw.dma("sp", modg_in, mrow[:], reads=[r_mrow], writes=[rs("modg_in")])
            allgather(modg_in, modg_out, G4, rs("modg_in"), rs("modg_out"))
        fw.barrier()

        def build_hT(st_outer, xsrc, ntok, hT, r_hT, tok0, tabA, tabB, r_tab, tag, tm_out=None, src_res=None):
            with contextlib.ExitStack() as st:
                xt = Rot([sb(st, tag + "xt%d" % i, [128, D], F32) for i in range(2)])
                junk = sb(st, tag + "junk", [128, D], BF16); r_junk = Res()
                xs = Rot([sb(st, tag + "xs%d" % i, [128, D], BF16) for i in range(2)])
                stat = Rot([sb(st, tag + "stat%d" % i, [128, 4], F32) for i in range(2)])
                ptr = Rot([ps(st, tag + "ptr%d" % i, [128, 1024], BF16) for i in range(2)])
                xv = xsrc.rearrange("(t p) d -> t p d", p=128)
                nt = ntok // 128
                for t in range(nt):
                    x_t, x_r = xt.next()
                    fw.dma("sp", x_t[:], xv[t], reads=([src_res] if src_res is not None else []), writes=[x_r])
                    s_t, s_r = stat.next()
                    fw.op("act", lambda: nc.scalar.activation(out=junk[:], in_=x_t[:], func=AF.Square, accum_out=s_t[:, 0:1]),
                          reads=[x_r], writes=[r_junk, s_r])
                    fw.op("act", lambda: nc.scalar.activation(out=s_t[:, 1:2], in_=s_t[:, 0:1], func=AF.Ln, scale=1.0 / D, bias=EPS),
                          reads=[s_r], writes=[s_r])
                    fw.op("act", lambda: nc.scalar.activation(out=s_t[:, 2:3], in_=s_t[:, 1:2], func=AF.Exp, scale=-0.5),
                          reads=[s_r], writes=[s_r])
                    fw.op("dve", lambda: nc.vector.scalar_tensor_tensor(out=x_t[:], in0=x_t[:], scalar=s_t[:, 2:3], in1=tabA[:], op0=ALU.mult, op1=ALU.mult),
                          reads=[s_r, r_tab], writes=[x_r])
                    xs_t, xs_r = xs.next()
                    fw.op("pool", lambda: nc.gpsimd.tensor_tensor(out=xs_t[:], in0=x_t[:], in1=tabB[:], op=ALU.add),
                          reads=[x_r, r_tab], writes=[xs_r])
                    if tm_out is not None:
                        fw.dma("sp", tm_out[t * 128:(t + 1) * 128, :], xs_t[:], reads=[xs_r], writes=[rs("tm_out")])
                    for g in range(4):
                        p_t, p_r = ptr.next()
                        for k in range(8):
                            c = g * 8 + k
                            fw.op("pe", lambda: nc.tensor.transpose(out=p_t[:, k * 128:(k + 1) * 128], in_=xs_t[:, c * 128:(c + 1) * 128], identity=ident_bf[:]),
                                  reads=[xs_r, r_ident], writes=[p_r], signal=(k == 7))
                        dst = hT[:, g * 8:(g + 1) * 8, tok0 + t * 128: tok0 + (t + 1) * 128]
                        src = p_t[:].rearrange("p (k t) -> p k t", k=8)
                        if g % 2 == 0:
                            fw.op("act", lambda: nc.scalar.copy(out=dst, in_=src), reads=[p_r], writes=[r_hT])
                        else:
                            fw.op("dve", lambda: nc.vector.tensor_copy(out=dst, in_=src), reads=[p_r], writes=[r_hT])

        def load_tabs(st, tabA, tabB, r_tab, row, seg_sh, seg_sc, nrow):
            with contextlib.ExitStack() as st2:
                nb = sb(st2, "nb_tmp", [128, D], F32); r_nb = Res()
                fw.dma("sp", nb[:], nrow[0, :].partition_broadcast(128), writes=[r_nb])
                mod_bc(tabA, seg_sc, row, r_tab)
                mod_bc(tabB, seg_sh, row, r_tab)
                fw.op("dve", lambda: nc.vector.scalar_tensor_tensor(out=tabA[:], in0=tabA[:], scalar=1.0, in1=nb[:], op0=ALU.add, op1=ALU.mult),
                      reads=[r_nb, r_tab], writes=[r_tab])
                fw.barrier()

        def stream_weights(wsrc_fn, nblocks, wt, r_wt, compute_fn):
            fw.dma("pool", wt[0][:], wsrc_fn(0), writes=[r_wt[0]])
            for b in range(nblocks):
                if b + 1 < nblocks:
                    s = (b + 1) % 2
                    fw.dma("pool", wt[s][:], wsrc_fn(b + 1), writes=[r_wt[s]])
                compute_fn(b, wt[b % 2], r_wt[b % 2])

        def mm_fm(p_t, p_r, w_t, w_r, wcol0, hT, r_hT, t0, tl, kc=KC):
            for c in range(kc):
                fw.op("pe", lambda: nc.tensor.matmul(p_t[:, 0:tl], lhsT=w_t[:, c, wcol0:wcol0 + 128], rhs=hT[:, c, t0:t0 + tl], start=(c == 0), stop=(c == kc - 1)),
                      reads=[w_r, r_hT], writes=[p_r], signal=(c == kc - 1))

        def mm_tm(p_t, p_r, w_t, w_r, wcol0, wn, hT, r_hT, t0, kc=KC):
            for c in range(kc):
                fw.op("pe", lambda: nc.tensor.matmul(p_t[:, 0:wn], lhsT=hT[:, c, t0:t0 + 128], rhs=w_t[:, c, wcol0:wcol0 + wn], start=(c == 0), stop=(c == kc - 1)),
                      reads=[w_r, r_hT], writes=[p_r], signal=(c == kc - 1))

        w_in_v = w_in.rearrange("(c p) n -> p c n", p=128) if w_in is not None else None
        aff_own = sb(top, "aff_own", [128, 8, 16], F32); r_aff = Res("aff_own")

        if "P1" in PHASES:
            with contextlib.ExitStack() as st:
                hT = sb(st, "hT_kvc", [128, KC, NKV + NCTX], BF16); r_hT = Res()
                with contextlib.ExitStack() as st1:
                    tabA = sb(st1, "tabA", [128, D], F32); tabB = sb(st1, "tabB", [128, D], F32); r_tab = Res()
                    load_tabs(st1, tabA, tabB, r_tab, 1, 0, 1, norm1_row)
                    build_hT(st1, ctxb, NCTX, hT, r_hT, NKV, tabA, tabB, r_tab, "c_")
                    fw.barrier()
                    load_tabs(st1, tabA, tabB, r_tab, 0, 0, 1, norm1_row)
                    build_hT(st1, x_kv, NKV, hT, r_hT, 0, tabA, tabB, r_tab, "k_")
                fw.barrier()
                WB = 256
                wt = [sb(st, "p1_wt%d" % i, [128, KC, WB], BF16) for i in range(2)]; r_wt = [Res(), Res()]
                pp = Rot([ps(st, "p1_ps%d" % i, [128, 512]) for i in range(4)])
                og = Rot([sb(st, "p1_o%d" % i, [128, 512], BF16) for i in range(4)])
                NT1 = NKV + NCTX
                blocks = [("nak", 2048 + i * WB) for i in range(2048 // WB)] + [("nav", 4096 + i * WB) for i in range(2048 // WB)] + \
                         [("rk", 8192 + i * WB) for i in range(2048 // WB)] + [("rv", 10240 + i * WB) for i in range(2048 // WB)]

                def p1_compute(b, w_t, w_r):
                    fam, col0 = blocks[b]
                    if fam == "nak":
                        for sub in range(WB // 128):
                            hd = (col0 - 2048) // 128 + sub
                            for t0 in range(0, NT1, 512):
                                tl = min(512, NT1 - t0)
                                p_t, p_r = pp.next()
                                mm_fm(p_t, p_r, w_t, w_r, sub * 128, hT, r_hT, t0, tl)
                                o_t, o_r = og.next()
                                fw.op("act", lambda: nc.scalar.copy(out=o_t[:, 0:tl], in_=p_t[:, 0:tl]), reads=[p_r], writes=[o_r])
                                fw.dma("sp", KaT[hd, :, t0:t0 + tl], o_t[:, 0:tl], reads=[o_r], writes=[rs("KaT")])
                    else:
                        toks = range(0, NT1, 128) if fam == "nav" else range(NKV, NT1, 128)
                        for t0 in toks:
                            p_t, p_r = pp.next()
                            mm_tm(p_t, p_r, w_t, w_r, 0, WB, hT, r_hT, t0)
                            o_t, o_r = og.next()
                            if fam == "rk":
                                fw.op("act", lambda: nc.scalar.mul(out=o_t[:, 0:WB], in_=p_t[:, 0:WB], mul=1.0 / 16.0), reads=[p_r], writes=[o_r])
                            else:
                                fw.op("dve", lambda: nc.vector.tensor_copy(out=o_t[:, 0:WB], in_=p_t[:, 0:WB]), reads=[p_r], writes=[o_r])
                            if fam == "nav":
                                fw.dma("sp", Va[t0:t0 + 128, col0 - 4096:col0 - 4096 + WB], o_t[:, 0:WB], reads=[o_r], writes=[rs("Va")])
                            elif fam == "rk":
                                fw.dma("sp", Krc[t0 - NKV:t0 - NKV + 128, col0 - 8192:col0 - 8192 + WB], o_t[:, 0:WB], reads=[o_r], writes=[rs("Krc")])
                            else:
                                fw.dma("sp", Vrc[t0 - NKV:t0 - NKV + 128, col0 - 10240:col0 - 10240 + WB], o_t[:, 0:WB], reads=[o_r], writes=[rs("Vrc")])

                stream_weights(lambda b: w_in_v[:, :, blocks[b][1]:blocks[b][1] + WB], len(blocks), wt, r_wt, p1_compute)
            fw.barrier()

        if "P2" in PHASES:
            with contextlib.ExitStack() as st:
                hT = sb(st, "hT_own", [128, KC, NTOK], BF16); r_hT = Res()
                with contextlib.ExitStack() as st1:
                    tabA = sb(st1, "tabA2", [128, D], F32); tabB = sb(st1, "tabB2", [128, D], F32); r_tab = Res()
                    load_tabs(st1, tabA, tabB, r_tab, 0, 0, 1, norm1_row)
                    build_hT(st1, x_own, NTOK, hT, r_hT, 0, tabA, tabB, r_tab, "o_")
                fw.barrier()
                WB = 512
                wt = [sb(st, "p2_wt%d" % i, [128, KC, WB], BF16) for i in range(2)]; r_wt = [Res(), Res()]
                pp = Rot([ps(st, "p2_ps%d" % i, [128, 512]) for i in range(4)])
                ptr = Rot([ps(st, "p2_ptr%d" % i, [128, 512], BF16) for i in range(2)])
                ob = Rot([sb(st, "p2_ob%d" % i, [128, 512], BF16) for i in range(4)])
                of = Rot([sb(st, "p2_of%d" % i, [128, 512], F32) for i in range(4)])
                otr = Rot([sb(st, "p2_otr%d" % i, [128, 512], BF16) for i in range(2)])
                tmp = Rot([sb(st, "p2_tmp%d" % i, [128, 4, 128], F32) for i in range(2)])
                rc = sb(st, "ropeC_sb", [128, 8, 128], F32); rsn = sb(st, "ropeS_sb", [128, 8, 128], F32)
                rck = sb(st, "ropeCk_sb", [128, 8, 128], F32); rsk = sb(st, "ropeSk_sb", [128, 8, 128], F32); r_rope = Res()
                for tsb, src in ((rc, ropeC), (rsn, ropeS), (rck, ropeCk), (rsk, ropeSk)):
                    fw.dma("sp", tsb[:], src.rearrange("(t p) f -> p t f", p=128), writes=[r_rope])
                blocks = [("naq", i * WB) for i in range(2048 // WB)] + [("rq", 6144 + i * WB) for i in range(2048 // WB)] + \
                         [("rk", 8192 + i * WB) for i in range(2048 // WB)] + [("rv", 10240 + i * WB) for i in range(2048 // WB)] + \
                         [("grf", 12288 + i * WB) for i in range(2048 // WB)] + [("grb", 14336 + i * WB) for i in range(2048 // WB)] + \
                         [("ga", 16384 + i * WB) for i in range(4096 // WB)] + [("gb", 20480 + i * WB) for i in range(4096 // WB)]

                def rope(p_t, p_r, tt, o_t, o_r, ctab, stab):
                    t_t, t_r = tmp.next()
                    for hh in range(2):
                        a = p_t[:, hh * 256:(hh + 1) * 256].rearrange("p (r h f) -> p r h f", r=2, h=2)
                        o = o_t[:, hh * 256:(hh + 1) * 256].rearrange("p (r h f) -> p r h f", r=2, h=2)
                        cs = ctab[:, tt, :].rearrange("p (r f) -> p r f", r=2)
                        sn = stab[:, tt, :].rearrange("p (r f) -> p r f", r=2)
                        a1 = a[:, :, 0, :]; a2 = a[:, :, 1, :]
                        T = [t_t[:, k, :].rearrange("p (r f) -> p r f", r=2) for k in range(4)]
                        fw.op("dve", lambda: nc.vector.tensor_tensor(out=T[0], in0=a1, in1=cs, op=ALU.mult), reads=[p_r, r_rope], writes=[t_r])
                        fw.op("dve", lambda: nc.vector.tensor_tensor(out=T[1], in0=a2, in1=sn, op=ALU.mult), reads=[p_r, r_rope], writes=[t_r])
                        fw.op("dve", lambda: nc.vector.tensor_tensor(out=T[2], in0=a1, in1=sn, op=ALU.mult), reads=[p_r, r_rope], writes=[t_r])
                        fw.op("dve", lambda: nc.vector.tensor_tensor(out=T[3], in0=a2, in1=cs, op=ALU.mult), reads=[p_r, r_rope], writes=[t_r])
                        fw.op("pool", lambda: nc.gpsimd.tensor_tensor(out=o[:, :, 0, :], in0=T[0], in1=T[1], op=ALU.subtract), reads=[t_r], writes=[o_r])
                        fw.op("pool", lambda: nc.gpsimd.tensor_tensor(out=o[:, :, 1, :], in0=T[2], in1=T[3], op=ALU.add), reads=[t_r], writes=[o_r])

                def p2_compute(b, w_t, w_r):
                    fam, col0 = blocks[b]
                    if fam in ("naq", "ga", "gb"):
                        for sub in range(WB // 128):
                            for t0 in range(0, NTOK, 512):
                                p_t, p_r = pp.next()
                                mm_fm(p_t, p_r, w_t, w_r, sub * 128, hT, r_hT, t0, 512)
                                if fam == "naq":
                                    hd = col0 // 128 + sub
                                    o_t, o_r = ob.next()
                                    fw.op("act", lambda: nc.scalar.copy(out=o_t[:], in_=p_t[:]), reads=[p_r], writes=[o_r])
                                    fw.dma("sp", QaT[hd, :, t0:t0 + 512], o_t[:], reads=[o_r], writes=[rs("QaT")])
                                else:
                                    base = 16384 if fam == "ga" else 20480
                                    dst = GaT if fam == "ga" else GbT
                                    ch = (col0 - base) // 128 + sub
                                    o_t, o_r = of.next()
                                    fw.op("act", lambda: nc.scalar.activation(out=o_t[:], in_=p_t[:], func=AF.Sigmoid), reads=[p_r], writes=[o_r])
                                    fw.dma("sp", dst[ch, :, t0:t0 + 512], o_t[:], reads=[o_r], writes=[rs("GaT" if fam == "ga" else "GbT")])
                    else:
                        for tt in range(NTOK // 128):
                            t0 = tt * 128
                            p_t, p_r = pp.next()
                            mm_tm(p_t, p_r, w_t, w_r, 0, WB, hT, r_hT, t0)
                            if fam in ("rq", "rk"):
                                o_t, o_r = ob.next()
                                base = 6144 if fam == "rq" else 8192
                                rope(p_t, p_r, tt, o_t, o_r, rc if fam == "rq" else rck, rsn if fam == "rq" else rsk)
                                c0 = col0 - base
                                if fam == "rk":
                                    fw.dma("sp", Kr[t0:t0 + 128, c0:c0 + WB], o_t[:], reads=[o_r], writes=[rs("Kr")])
                                q_t, q_r = ptr.next()
                                for k in range(4):
                                    fw.op("pe", lambda: nc.tensor.transpose(out=q_t[:, k * 128:(k + 1) * 128], in_=o_t[:, k * 128:(k + 1) * 128], identity=ident_bf[:]),
                                          reads=[o_r, r_ident], writes=[q_r], signal=(k == 3))
                                x_t, x_r = otr.next()
                                fw.op("act", lambda: nc.scalar.copy(out=x_t[:], in_=q_t[:]), reads=[q_r], writes=[x_r])
                                dstT = QrT if fam == "rq" else KrT
                                h0 = c0 // 256
                                for hh in range(2):
                                    fw.dma("sp", dstT[h0 + hh, :, :, t0:t0 + 128],
                                           x_t[:, hh * 256:(hh + 1) * 256].rearrange("p (c t) -> p c t", c=2), reads=[x_r], writes=[rs("QrT" if fam == "rq" else "KrT")])
                            elif fam == "rv":
                                o_t, o_r = ob.next()
                                fw.op("act", lambda: nc.scalar.copy(out=o_t[:], in_=p_t[:]), reads=[p_r], writes=[o_r])
                                fw.dma("sp", Vr[t0:t0 + 128, col0 - 10240:col0 - 10240 + WB], o_t[:], reads=[o_r], writes=[rs("Vr")])
                            else:
                                o_t, o_r = of.next()
                                fw.op("act", lambda: nc.scalar.activation(out=o_t[:], in_=p_t[:], func=AF.Silu), reads=[p_r], writes=[o_r])
                                base = 12288 if fam == "grf" else 14336
                                dst = Grf if fam == "grf" else Grb
                                fw.dma("sp", dst[t0:t0 + 128, col0 - base:col0 - base + WB], o_t[:], reads=[o_r], writes=[rs("Grf" if fam == "grf" else "Grb")])

                stream_weights(lambda b: w_in_v[:, :, blocks[b][1]:blocks[b][1] + WB], len(blocks), wt, r_wt, p2_compute)
            fw.barrier()


        if "NA" in PHASES:
            with contextlib.ExitStack() as st:
                oneA = sb(st, "oneA", [128, NKV], BF16); vB = sb(st, "vB", [128, NTOK], BF16); r_mask = Res()
                fw.dma("pool", oneA[:], onehotA, writes=[r_mask]); fw.dma("pool", vB[:], validB, writes=[r_mask])
                ones_bf = sb(st, "ones_bf", [128, 128], BF16); r_ones = Res()
                fw.op("dve", lambda: nc.vector.memset(ones_bf[:], 1.0), writes=[r_ones])
                kTr = Rot([sb(st, "na_kT%d" % i, [128, NKV + NCTX], BF16) for i in range(2)])
                vAr = Rot([sb(st, "na_vA%d" % i, [128, 14, 128], BF16) for i in range(2)])
                qTr = Rot([sb(st, "na_qT%d" % i, [128, NTOK], BF16) for i in range(2)])
                bfr = Rot([sb(st, "na_bf%d" % i, [128, 896], F32) for i in range(2)])
                ebr = Rot([sb(st, "na_eb%d" % i, [128, 896], BF16) for i in range(2)])
                yTr = Rot([sb(st, "na_yT%d" % i, [128, NTOK], BF16) for i in range(2)])
                Pr = Rot([sb(st, "na_P%d" % i, [128, 1024], BF16) for i in range(2)])
                rdr = Rot([sb(st, "na_rd%d" % i, [128, 128], F32) for i in range(2)])
                Sps = Rot([ps(st, "na_S%d" % i, [128, 1024]) for i in range(2)])
                Ops = Rot([ps(st, "na_O%d" % i, [128, 256]) for i in range(2)])
                Va_v = Va.rearrange("(t p) c -> p t c", p=128)
                SC = 128 ** -0.5
                for h in range(16):
                    kT, kT_r = kTr.next(); vA, vA_r = vAr.next(); qT, qT_r = qTr.next()
                    bf, bf_r = bfr.next(); eb, eb_r = ebr.next(); yT, yT_r = yTr.next()
                    fw.dma("sp", kT[:], KaT[h], reads=[rs("KaT")], writes=[kT_r])
                    fw.dma("sp", vA[:], Va_v[:, :, h * 128:(h + 1) * 128], reads=[rs("Va")], writes=[vA_r])
                    fw.dma("sp", qT[:], QaT[h], reads=[rs("QaT")], writes=[qT_r])
                    fw.dma("sp", bf[:], nabias[h], writes=[bf_r])
                    fw.op("act", lambda: nc.scalar.activation(out=eb[:], in_=bf[:], func=AF.Exp), reads=[bf_r], writes=[eb_r])
                    for pr in range(8):
                        if pr == 0:
                            kts = list(range(0, 6)); idx0 = 1
                        elif pr == 7:
                            kts = list(range(6, 12)); idx0 = 0
                        else:
                            kts = list(range(pr, pr + 5)); idx0 = 1
                        nw = len(kts); nt = nw + 2
                        S_t, S_r = Sps.next()
                        q_ap = qT[:, pr * 128:(pr + 1) * 128]
                        for i, kt in enumerate(kts):
                            fw.op("pe", lambda: nc.tensor.matmul(S_t[:, i * 128:(i + 1) * 128], lhsT=kT[:, kt * 128:(kt + 1) * 128], rhs=q_ap, start=True, stop=False),
                                  reads=[kT_r, qT_r], writes=[S_r], signal=False)
                            fw.op("pe", lambda: nc.tensor.matmul(S_t[:, i * 128:(i + 1) * 128], lhsT=oneA[:, kt * 128:(kt + 1) * 128], rhs=vB[:, pr * 128:(pr + 1) * 128], start=False, stop=True),
                                  reads=[r_mask], writes=[S_r], signal=False)
                        for i in range(2):
                            fw.op("pe", lambda: nc.tensor.matmul(S_t[:, (nw + i) * 128:(nw + i + 1) * 128], lhsT=kT[:, NKV + i * 128:NKV + (i + 1) * 128], rhs=q_ap, start=True, stop=True),
                                  reads=[kT_r, qT_r], writes=[S_r], signal=(i == 1))
                        P_t, P_r = Pr.next()
                        fw.op("act", lambda: nc.scalar.activation(out=P_t[:, 0:512], in_=S_t[:, 0:512], func=AF.Exp, scale=SC), reads=[S_r], writes=[P_r])
                        fw.op("act", lambda: nc.scalar.activation(out=P_t[:, 512:nt * 128], in_=S_t[:, 512:nt * 128], func=AF.Exp, scale=SC), reads=[S_r], writes=[P_r])
                        fw.op("dve", lambda: nc.vector.tensor_tensor(out=P_t[:, 0:nw * 128], in0=P_t[:, 0:nw * 128], in1=eb[:, idx0 * 128:(idx0 + nw) * 128], op=ALU.mult),
                              reads=[eb_r, P_r], writes=[P_r])
                        O_t, O_r = Ops.next()
                        vts = kts + [12, 13]
                        for i in range(nt):
                            fw.op("pe", lambda: nc.tensor.matmul(O_t[:, 0:128], lhsT=vA[:, vts[i], :], rhs=P_t[:, i * 128:(i + 1) * 128], start=(i == 0), stop=(i == nt - 1)),
                                  reads=[vA_r, P_r], writes=[O_r], signal=False)
                        for i in range(nt):
                            fw.op("pe", lambda: nc.tensor.matmul(O_t[:, 128:256], lhsT=ones_bf[:], rhs=P_t[:, i * 128:(i + 1) * 128], start=(i == 0), stop=(i == nt - 1)),
                                  reads=[r_ones, P_r], writes=[O_r], signal=(i == nt - 1))
                        rd, rd_r = rdr.next()
                        fw.op("dve", lambda: nc.vector.reciprocal(out=rd[:], in_=O_t[:, 128:256]), reads=[O_r], writes=[rd_r])
                        fw.op("dve", lambda: nc.vector.tensor_tensor(out=yT[:, pr * 128:(pr + 1) * 128], in0=O_t[:, 0:128], in1=rd[:], op=ALU.mult),
                              reads=[O_r, rd_r], writes=[yT_r])
                    fw.dma("pool", ynaT[h], yT[:], reads=[yT_r], writes=[rs("ynaT")])
            fw.barrier()

        if "R" in PHASES:
            with contextlib.ExitStack() as st:
                rd16 = sb(st, "rd16", [128, 16], F32); lg = sb(st, "lg", [128, 16], F32); r_lg = Res()
                Mt = sb(st, "Mt", [128, 256], F32); pos = sb(st, "pos", [128, 26], F32); r_tb = Res()
                cE = sb(st, "cE", [128, 10], F32); cM = sb(st, "cM", [128, 10], F32)
                Dt = sb(st, "Dt", [128, 16, 128], F32); ex = sb(st, "ex", [128, 16, 13], F32); cco = sb(st, "cco", [128, 16, 5], F32); r_ex = Res()
                fw.dma("sp", rd16[:], ret_decay[0, :].partition_broadcast(128), writes=[r_lg])
                fw.dma("sp", Mt[:], retM, writes=[r_tb]); fw.dma("sp", pos[:], retpos, writes=[r_tb])
                fw.dma("sp", cE[:], ccE[0, :].partition_broadcast(128), writes=[r_tb]); fw.dma("sp", cM[:], ccM[0, :].partition_broadcast(128), writes=[r_tb])
                fw.op("act", lambda: nc.scalar.activation(out=rd16[:], in_=rd16[:], func=AF.Exp, scale=-0.6931471805599453), reads=[r_lg], writes=[r_lg])
                fw.op("act", lambda: nc.scalar.activation(out=lg[:], in_=rd16[:], func=AF.Ln, scale=-1.0, bias=1.0), reads=[r_lg], writes=[r_lg])
                for k in range(16):
                    d = k // 8
                    fw.op("dve", lambda: nc.vector.tensor_scalar(out=Dt[:, k, :], in0=Mt[:, d * 128:(d + 1) * 128], scalar1=lg[:, k:k + 1], scalar2=None, op0=ALU.mult),
                          reads=[r_lg, r_tb], writes=[r_ex])
                    fw.op("dve", lambda: nc.vector.tensor_scalar(out=ex[:, k, :], in0=pos[:, d * 13:(d + 1) * 13], scalar1=lg[:, k:k + 1], scalar2=None, op0=ALU.mult),
                          reads=[r_lg, r_tb], writes=[r_ex])
                    fw.op("dve", lambda: nc.vector.tensor_scalar(out=cco[:, k, :], in0=cE[:, d * 5:(d + 1) * 5], scalar1=lg[:, k:k + 1], scalar2=None, op0=ALU.mult),
                          reads=[r_lg, r_tb], writes=[r_ex])
                fw.op("act", lambda: nc.scalar.activation(out=Dt[:], in_=Dt[:], func=AF.Exp), reads=[r_ex], writes=[r_ex])
                fw.op("act", lambda: nc.scalar.activation(out=ex[:], in_=ex[:], func=AF.Exp), reads=[r_ex], writes=[r_ex])
                fw.op("act", lambda: nc.scalar.activation(out=cco[:], in_=cco[:], func=AF.Exp), reads=[r_ex], writes=[r_ex])
                for k in range(16):
                    d = k // 8
                    fw.op("dve", lambda: nc.vector.tensor_tensor(out=cco[:, k, :], in0=cco[:, k, :], in1=cM[:, d * 5:(d + 1) * 5], op=ALU.mult), reads=[r_ex, r_tb], writes=[r_ex])
                Kr_v = Kr.rearrange("(t p) c -> p t c", p=128); Vr_v = Vr.rearrange("(t p) c -> p t c", p=128)
                Krc_v = Krc.rearrange("(t p) c -> p t c", p=128); Vrc_v = Vrc.rearrange("(t p) c -> p t c", p=128)
                Grf_v = Grf.rearrange("(t p) c -> p t c", p=128); Grb_v = Grb.rearrange("(t p) c -> p t c", p=128)
                Sf = sb(st, "Sf", [128, 32, 256], F32); Sb_ = sb(st, "Sbf", [128, 32, 256], BF16)
                r_S = [Res() for _ in range(32)]
                with contextlib.ExitStack() as st1:
                    Khr = Rot([sb(st1, "r1_K%d" % i, [128, 8, 256], BF16) for i in range(2)])
                    Vhr = Rot([sb(st1, "r1_V%d" % i, [128, 8, 256], BF16) for i in range(2)])
                    Kcr = Rot([sb(st1, "r1_Kc%d" % i, [128, 2, 256], BF16) for i in range(2)])
                    Vcr = Rot([sb(st1, "r1_Vc%d" % i, [128, 2, 256], BF16) for i in range(2)])
                    Kwr = Rot([sb(st1, "r1_Kw%d" % i, [128, 10, 256], BF16) for i in range(2)])
                    Lor = Rot([sb(st1, "r1_Lo%d" % i, [128, 256], F32) for i in range(4)])
                    Lps = Rot([ps(st1, "r1_ps%d" % i, [128, 256]) for i in range(4)])
                    for h in range(8):
                        Kh, Kh_r = Khr.next(); Vh, Vh_r = Vhr.next(); Kc, Kc_r = Kcr.next(); Vc, Vc_r = Vcr.next()
                        fw.dma("sp", Kh[:], Kr_v[:, :, h * 256:(h + 1) * 256], reads=[rs("Kr")], writes=[Kh_r])
                        fw.dma("sp", Vh[:], Vr_v[:, :, h * 256:(h + 1) * 256], reads=[rs("Vr")], writes=[Vh_r])
                        fw.dma("sp", Kc[:], Krc_v[:, :, h * 256:(h + 1) * 256], reads=[rs("Krc")], writes=[Kc_r])
                        fw.dma("sp", Vc[:], Vrc_v[:, :, h * 256:(h + 1) * 256], reads=[rs("Vrc")], writes=[Vc_r])
                        for d in range(2):
                            k = d * 8 + h
                            Kw, Kw_r = Kwr.next()
                            for t in range(8):
                                eng = "dve" if t % 2 == 0 else "pool"
                                E_ = nc.vector if eng == "dve" else nc.gpsimd
                                fw.op(eng, lambda: E_.tensor_scalar(out=Kw[:, t, :], in0=Kh[:, t, :], scalar1=ex[:, k, 3 + t:4 + t], scalar2=None, op0=ALU.mult),
                                      reads=[Kh_r, r_ex], writes=[Kw_r])
                            for t in range(2):
                                fw.op("dve", lambda: nc.vector.tensor_scalar(out=Kw[:, 8 + t, :], in0=Kc[:, t, :], scalar1=ex[:, k, 11 + t:12 + t], scalar2=None, op0=ALU.mult),
                                      reads=[Kc_r, r_ex], writes=[Kw_r])
                            for dkc in range(2):
                                row0 = (k * 2 + dkc) * 128
                                p_t, p_r = Lps.next()
                                for t in range(8):
                                    fw.op("pe", lambda: nc.tensor.matmul(p_t[:], lhsT=Kw[:, t, dkc * 128:(dkc + 1) * 128], rhs=Vh[:, t, :], start=(t == 0), stop=(t == 7)),
                                          reads=[Kw_r, Vh_r], writes=[p_r], signal=(t == 7))
                                o_t, o_r = Lor.next()
                                fw.op("act", lambda: nc.scalar.copy(out=o_t[:], in_=p_t[:]), reads=[p_r], writes=[o_r])
                                fw.dma("sp", Lst_in[row0:row0 + 128, :], o_t[:], reads=[o_r], writes=[rs("Lst_in")])
                                p_t, p_r = Lps.next()
                                for t in range(2):
                                    fw.op("pe", lambda: nc.tensor.matmul(p_t[:], lhsT=Kw[:, 8 + t, dkc * 128:(dkc + 1) * 128], rhs=Vc[:, t, :], start=(t == 0), stop=(t == 1)),
                                          reads=[Kw_r, Vc_r], writes=[p_r], signal=(t == 1))
                                o_t, o_r = Lor.next()
                                fw.op("act", lambda: nc.scalar.copy(out=o_t[:], in_=p_t[:]), reads=[p_r], writes=[o_r])
                                fw.dma("sp", S0d[row0:row0 + 128, :], o_t[:], reads=[o_r], writes=[rs("S0d")])
                    allgather_chunks(Lst_in, Lst_out, G4, 4, rs("Lst_in"), rs("Lst_out"))
                    Lar = Rot([sb(st1, "r2_La%d" % i, [128, 5, 256], F32) for i in range(2)])
                    Lo_v = Lst_out.rearrange("(c i q p) v -> p c i q v", c=4, i=4, p=128)
                    for k in range(16):
                        for dkc in range(2):
                            q = k * 2 + dkc
                            La, La_r = Lar.next()
                            fw.dma("sp", La[:, 0:4, :], Lo_v[:, q // 8, :, q % 8, :], reads=[rs("Lst_out")], writes=[La_r])
                            fw.dma("sp", La[:, 4, :], S0d[q * 128:(q + 1) * 128, :], reads=[rs("S0d")], writes=[La_r])
                            fw.op("dve", lambda: nc.vector.tensor_scalar(out=Sf[:, q, :], in0=La[:, 0, :], scalar1=cco[:, k, 0:1], scalar2=None, op0=ALU.mult),
                                  reads=[La_r, r_ex], writes=[r_S[q]])
                            for i in range(1, 5):
                                fw.op("dve", lambda: nc.vector.scalar_tensor_tensor(out=Sf[:, q, :], in0=La[:, i, :], scalar=cco[:, k, i:i + 1], in1=Sf[:, q, :], op0=ALU.mult, op1=ALU.add),
                                      reads=[La_r, r_ex], writes=[r_S[q]])
                            fw.op("act", lambda: nc.scalar.copy(out=Sb_[:, q, :], in_=Sf[:, q, :]), reads=[r_S[q]], writes=[r_S[q]])
                fw.barrier()
                with contextlib.ExitStack() as st1:
                    QTr = Rot([sb(st1, "r3_QT%d" % i, [128, 2, NTOK], BF16) for i in range(2)])
                    KTr = Rot([sb(st1, "r3_KT%d" % i, [128, 2, NTOK], BF16) for i in range(2)])
                    Khr = Rot([sb(st1, "r3_K%d" % i, [128, 8, 256], BF16) for i in range(2)])
                    Vhr = Rot([sb(st1, "r3_V%d" % i, [128, 8, 256], BF16) for i in range(2)])
                    Gfr = Rot([sb(st1, "r3_Gf%d" % i, [128, 8, 256], F32) for i in range(2)])
                    Gbr = Rot([sb(st1, "r3_Gb%d" % i, [128, 8, 256], F32) for i in range(2)])
                    yaccr = Rot([sb(st1, "r3_ya%d" % i, [128, 8, 256], F32) for i in range(2)])
                    ybfr = Rot([sb(st1, "r3_yb%d" % i, [128, 8, 256], BF16) for i in range(2)])
                    Ar = Rot([sb(st1, "r3_A%d" % i, [128, 128], BF16) for i in range(2)])
                    o1r = Rot([sb(st1, "r3_o1%d" % i, [128, 256], F32) for i in range(2)])
                    osr = Rot([sb(st1, "r3_os%d" % i, [128, 256], F32) for i in range(2)])
                    tmr = Rot([sb(st1, "r3_tm%d" % i, [128, 256], F32) for i in range(2)])
                    jkr = Rot([sb(st1, "r3_jk%d" % i, [128, 256], BF16) for i in range(2)])
                    str_ = Rot([sb(st1, "r3_st%d" % i, [128, 4], F32) for i in range(4)])
                    Kdr = Rot([sb(st1, "r3_Kd%d" % i, [128, 256], BF16) for i in range(2)])
                    yor = Rot([sb(st1, "r3_yo%d" % i, [128, NTOK], BF16) for i in range(2)])
                    pA = Rot([ps(st1, "r3_pA%d" % i, [128, 128]) for i in range(2)])
                    pO = Rot([ps(st1, "r3_pO%d" % i, [128, 256]) for i in range(3)])
                    pS = Rot([ps(st1, "r3_pS%d" % i, [128, 256]) for i in range(2)])
                    pT = Rot([ps(st1, "r3_pT%d" % i, [128, NTOK], BF16) for i in range(1)])
                    for h in range(8):
                        QT, QT_r = QTr.next(); KT, KT_r = KTr.next(); Kh, Kh_r = Khr.next(); Vh, Vh_r = Vhr.next()
                        Gf, Gf_r = Gfr.next(); Gb, Gb_r = Gbr.next(); ya, ya_r = yaccr.next()
                        fw.dma("sp", QT[:], QrT[h], reads=[rs("QrT")], writes=[QT_r])
                        fw.dma("sp", KT[:], KrT[h], reads=[rs("KrT")], writes=[KT_r])
                        fw.dma("sp", Kh[:], Kr_v[:, :, h * 256:(h + 1) * 256], reads=[rs("Kr")], writes=[Kh_r])
                        fw.dma("sp", Vh[:], Vr_v[:, :, h * 256:(h + 1) * 256], reads=[rs("Vr")], writes=[Vh_r])
                        fw.dma("sp", Gf[:], Grf_v[:, :, h * 256:(h + 1) * 256], reads=[rs("Grf")], writes=[Gf_r])
                        fw.dma("sp", Gb[:], Grb_v[:, :, h * 256:(h + 1) * 256], reads=[rs("Grb")], writes=[Gb_r])
                        for oi in range(8):
                          for d in range(2):
                            k = d * 8 + h
                            n = oi if d == 0 else 7 - oi
                            first = (n <= 3) if d == 0 else (n >= 4)
                            G_, G_r = (Gf, Gf_r) if d == 0 else (Gb, Gb_r)
                            for _once in (0,):
                                ch = slice(n * 128, (n + 1) * 128)
                                a_t, a_r = pA.next()
                                for dkc in range(2):
                                    fw.op("pe", lambda: nc.tensor.matmul(a_t[:], lhsT=KT[:, dkc, ch], rhs=QT[:, dkc, ch], start=(dkc == 0), stop=(dkc == 1)),
                                          reads=[KT_r, QT_r], writes=[a_r], signal=(dkc == 1))
                                A_t, A_r = Ar.next()
                                fw.op("dve", lambda: nc.vector.tensor_tensor(out=A_t[:], in0=a_t[:], in1=Dt[:, k, :], op=ALU.mult), reads=[a_r, r_ex], writes=[A_r])
                                o1_t, o1_r = pO.next()
                                fw.op("pe", lambda: nc.tensor.matmul(o1_t[:], lhsT=A_t[:], rhs=Vh[:, n, :], start=True, stop=True), reads=[A_r, Vh_r], writes=[o1_r])
                                o2_t, o2_r = pO.next()
                                for dkc in range(2):
                                    q = k * 2 + dkc
                                    fw.op("pe", lambda: nc.tensor.matmul(o2_t[:], lhsT=QT[:, dkc, ch], rhs=Sb_[:, q, :], start=(dkc == 0), stop=(dkc == 1)),
                                          reads=[QT_r, r_S[q]], writes=[o2_r], signal=(dkc == 1))
                                o1s, o1s_r = o1r.next()
                                fw.op("act", lambda: nc.scalar.copy(out=o1s[:], in_=o1_t[:]), reads=[o1_r], writes=[o1s_r])
                                os_, os_r = osr.next()
                                fw.op("dve", lambda: nc.vector.scalar_tensor_tensor(out=os_[:], in0=o2_t[:], scalar=ex[:, k, 0:1], in1=o1s[:], op0=ALU.mult, op1=ALU.add),
                                      reads=[o2_r, o1s_r, r_ex], writes=[os_r])
                                s_t, s_r = str_.next(); jk, jk_r = jkr.next()
                                fw.op("act", lambda: nc.scalar.activation(out=jk[:], in_=os_[:], func=AF.Square, accum_out=s_t[:, 0:1]), reads=[os_r], writes=[jk_r, s_r])
                                fw.op("act", lambda: nc.scalar.activation(out=s_t[:, 1:2], in_=s_t[:, 0:1], func=AF.Ln, scale=1.0 / 256, bias=EPS), reads=[s_r], writes=[s_r])
                                fw.op("act", lambda: nc.scalar.activation(out=s_t[:, 2:3], in_=s_t[:, 1:2], func=AF.Exp, scale=-0.5), reads=[s_r], writes=[s_r])
                                if first:
                                    fw.op("dve", lambda: nc.vector.scalar_tensor_tensor(out=ya[:, n, :], in0=os_[:], scalar=s_t[:, 2:3], in1=G_[:, n, :], op0=ALU.mult, op1=ALU.mult),
                                          reads=[os_r, s_r, G_r], writes=[ya_r])
                                else:
                                    tm, tm_r = tmr.next()
                                    fw.op("dve", lambda: nc.vector.scalar_tensor_tensor(out=tm[:], in0=os_[:], scalar=s_t[:, 2:3], in1=G_[:, n, :], op0=ALU.mult, op1=ALU.mult),
                                          reads=[os_r, s_r, G_r], writes=[tm_r])
                                    fw.op("pool", lambda: nc.gpsimd.tensor_tensor(out=ya[:, n, :], in0=ya[:, n, :], in1=tm[:], op=ALU.add), reads=[tm_r, ya_r], writes=[ya_r])
                                if oi < 7:
                                    Kd, Kd_r = Kdr.next()
                                    fw.op("pool", lambda: nc.gpsimd.tensor_scalar(out=Kd[:], in0=Kh[:, n, :], scalar1=ex[:, k, 1:2], scalar2=None, op0=ALU.mult), reads=[Kh_r, r_ex], writes=[Kd_r])
                                    for dkc in range(2):
                                        q = k * 2 + dkc
                                        dS, dS_r = pS.next()
                                        fw.op("pe", lambda: nc.tensor.matmul(dS[:], lhsT=Kd[:, dkc * 128:(dkc + 1) * 128], rhs=Vh[:, n, :], start=True, stop=True), reads=[Kd_r, Vh_r], writes=[dS_r])
                                        fw.op("dve", lambda: nc.vector.scalar_tensor_tensor(out=Sf[:, q, :], in0=Sf[:, q, :], scalar=ex[:, k, 2:3], in1=dS[:], op0=ALU.mult, op1=ALU.add),
                                              reads=[dS_r, r_ex, r_S[q]], writes=[r_S[q]])
                                        fw.op("act", lambda: nc.scalar.copy(out=Sb_[:, q, :], in_=Sf[:, q, :]), reads=[r_S[q]], writes=[r_S[q]])
                        yb, yb_r = ybfr.next()
                        fw.op("act", lambda: nc.scalar.copy(out=yb[:], in_=ya[:]), reads=[ya_r], writes=[yb_r])
                        for c2 in range(2):
                            p_t, p_r = pT.next()
                            for n in range(8):
                                fw.op("pe", lambda: nc.tensor.transpose(out=p_t[:, n * 128:(n + 1) * 128], in_=yb[:, n, c2 * 128:(c2 + 1) * 128], identity=ident_bf[:]),
                                      reads=[yb_r, r_ident], writes=[p_r], signal=(n == 7))
                            yo, yo_r = yor.next()
                            fw.op("dve", lambda: nc.vector.tensor_copy(out=yo[:], in_=p_t[:]), reads=[p_r], writes=[yo_r])
                            fw.dma("sp", yretT[h * 2 + c2], yo[:], reads=[yo_r], writes=[rs("yretT")])
            fw.barrier()

        if "G" in PHASES:
            with contextlib.ExitStack() as st:
                mixT = sb(st, "mixT", [128, KC, NTOK], BF16); r_mix = Res()
                with contextlib.ExitStack() as st1:
                    ynT = sb(st1, "ynT", [128, 16, NTOK], BF16); yrT = sb(st1, "yrT", [128, 16, NTOK], BF16); r_yn = Res(); r_yr = Res()
                    fw.dma("sp", ynT[:], ynaT.rearrange("c p t -> p c t"), reads=[rs("ynaT")], writes=[r_yn])
                    fw.dma("sp", yrT[:], yretT.rearrange("c p t -> p c t"), reads=[rs("yretT")], writes=[r_yr])
                    WB = 128
                    wa = [sb(st1, "g_wa%d" % i, [128, 16, WB], BF16) for i in range(2)]; r_wa = [Res(), Res()]
                    wb = [sb(st1, "g_wb%d" % i, [128, 16, WB], BF16) for i in range(2)]; r_wb = [Res(), Res()]
                    gar = Rot([sb(st1, "g_ga%d" % i, [128, 512], F32) for i in range(2)])
                    gbr = Rot([sb(st1, "g_gb%d" % i, [128, 512], F32) for i in range(2)])
                    t1r = Rot([sb(st1, "g_t1%d" % i, [128, 512], F32) for i in range(2)])
                    t2r = Rot([sb(st1, "g_t2%d" % i, [128, 512], F32) for i in range(2)])
                    pp = Rot([ps(st1, "g_ps%d" % i, [128, 512]) for i in range(4)])
                    wa_v = w_bna.rearrange("(c p) n -> p c n", p=128); wb_v = w_bret.rearrange("(c p) n -> p c n", p=128)
                    nb = D // WB
                    fw.dma("pool", wa[0][:], wa_v[:, :, 0:WB], writes=[r_wa[0]]); fw.dma("pool", wb[0][:], wb_v[:, :, 0:WB], writes=[r_wb[0]])
                    for b in range(nb):
                        if b + 1 < nb:
                            s_ = (b + 1) % 2
                            fw.dma("pool", wa[s_][:], wa_v[:, :, (b + 1) * WB:(b + 2) * WB], writes=[r_wa[s_]])
                            fw.dma("pool", wb[s_][:], wb_v[:, :, (b + 1) * WB:(b + 2) * WB], writes=[r_wb[s_]])
                        ch = b
                        for t0 in (0, 512):
                            ga, ga_r = gar.next(); gb, gb_r = gbr.next()
                            fw.dma("sp", ga[:], GaT[ch, :, t0:t0 + 512], reads=[rs("GaT")], writes=[ga_r])
                            fw.dma("sp", gb[:], GbT[ch, :, t0:t0 + 512], reads=[rs("GbT")], writes=[gb_r])
                            pa, pa_r = pp.next()
                            mm_fm(pa, pa_r, wa[b % 2], r_wa[b % 2], 0, ynT, r_yn, t0, 512, kc=16)
                            pb, pb_r = pp.next()
                            mm_fm(pb, pb_r, wb[b % 2], r_wb[b % 2], 0, yrT, r_yr, t0, 512, kc=16)
                            t1, t1_r = t1r.next(); t2, t2_r = t2r.next()
                            fw.op("dve", lambda: nc.vector.tensor_tensor(out=t1[:], in0=pa[:], in1=ga[:], op=ALU.mult), reads=[pa_r, ga_r], writes=[t1_r])
                            fw.op("dve", lambda: nc.vector.tensor_tensor(out=t2[:], in0=pb[:], in1=gb[:], op=ALU.mult), reads=[pb_r, gb_r], writes=[t2_r])
                            fw.op("pool", lambda: nc.gpsimd.tensor_tensor(out=mixT[:, ch, t0:t0 + 512], in0=t1[:], in1=t2[:], op=ALU.add), reads=[t1_r, t2_r], writes=[r_mix])
                fw.barrier()
                with contextlib.ExitStack() as st1:
                    WB = 512
                    wt = [sb(st1, "go_wt%d" % i, [128, KC, WB], BF16) for i in range(2)]; r_wt = [Res(), Res()]
                    g1bc = sb(st1, "g1bc", [128, D], F32); r_g1 = Res()
                    mod_bc(g1bc, 2, 0, r_g1)
                    xor_ = Rot([sb(st1, "go_x%d" % i, [128, 512], F32) for i in range(3)])
                    tr_ = Rot([sb(st1, "go_t%d" % i, [128, 512], F32) for i in range(3)])
                    pp = Rot([ps(st1, "go_ps%d" % i, [128, 512]) for i in range(4)])
                    wo_v = w_out.rearrange("(c p) n -> p c n", p=128)

                    def go_compute(b, w_t, w_r):
                        for tt in range(8):
                            xo, xo_r = xor_.next()
                            fw.dma("sp", xo[:], x_own[tt * 128:(tt + 1) * 128, b * 512:(b + 1) * 512], writes=[xo_r])
                            p_t, p_r = pp.next()
                            mm_tm(p_t, p_r, w_t, w_r, 0, 512, mixT, r_mix, tt * 128)
                            t_, t_r = tr_.next()
                            fw.op("dve", lambda: nc.vector.tensor_tensor(out=t_[:], in0=p_t[:], in1=g1bc[:, b * 512:(b + 1) * 512], op=ALU.mult), reads=[p_r, r_g1], writes=[t_r])
                            fw.op("pool", lambda: nc.gpsimd.tensor_tensor(out=t_[:], in0=t_[:], in1=xo[:], op=ALU.add), reads=[xo_r, t_r], writes=[t_r])
                            fw.dma("sp", Xmid[tt * 128:(tt + 1) * 128, b * 512:(b + 1) * 512], t_[:], reads=[t_r], writes=[rs("Xmid")])

                    stream_weights(lambda b: wo_v[:, :, b * 512:(b + 1) * 512], 8, wt, r_wt, go_compute)
            fw.barrier()

        if "N2" in PHASES:
            with contextlib.ExitStack() as st:
                h2T = sb(st, "h2T", [128, KC, NTOK], BF16); r_h2T = Res()
                with contextlib.ExitStack() as st1:
                    tabA = sb(st1, "tabA3", [128, D], F32); tabB = sb(st1, "tabB3", [128, D], F32); r_tab = Res()
                    load_tabs(st1, tabA, tabB, r_tab, 0, 3, 4, norm2_row)
                    build_hT(st1, Xmid, NTOK, h2T, r_h2T, 0, tabA, tabB, r_tab, "n_", tm_out=h2_in, src_res=rs("Xmid"))
                fw.barrier()
                allgather_chunks(h2_in, h2_all, G4, 8, rs("tm_out"), rs("h2_all"))
                wr = sb(st, "wr", [128, KC, 16], BF16); r_wr = Res()
                fw.dma("pool", wr[:], w_router.rearrange("(c p) e -> p c e", p=128), writes=[r_wr])
                lpr = Rot([ps(st, "n2_lp%d" % i, [128, 16]) for i in range(2)])
                sm = Rot([sb(st, "n2_sm%d" % i, [128, 4], F32) for i in range(2)])
                et = Rot([sb(st, "n2_et%d" % i, [128, 16], F32) for i in range(2)])
                for tt in range(8):
                    lp, lp_r = lpr.next()
                    for c in range(KC):
                        fw.op("pe", lambda: nc.tensor.matmul(lp, lhsT=h2T[:, c, tt * 128:(tt + 1) * 128], rhs=wr[:, c, :], start=(c == 0), stop=(c == KC - 1)),
                              reads=[r_h2T, r_wr], writes=[lp_r], signal=(c == KC - 1))
                    s_t, s_r = sm.next(); e_t, e_r = et.next()
                    fw.op("dve", lambda: nc.vector.reduce_max(out=s_t[:, 0:1], in_=lp, axis=AX.X), reads=[lp_r], writes=[s_r])
                    fw.op("dve", lambda: nc.vector.tensor_scalar(out=s_t[:, 1:2], in0=s_t[:, 0:1], scalar1=-1.0, scalar2=None, op0=ALU.mult), reads=[s_r], writes=[s_r])
                    fw.op("act", lambda: nc.scalar.activation(out=e_t[:], in_=lp, func=AF.Exp, bias=s_t[:, 1:2], scale=1.0, accum_out=s_t[:, 2:3]), reads=[lp_r, s_r], writes=[e_r, s_r])
                    fw.op("dve", lambda: nc.vector.reciprocal(out=s_t[:, 3:4], in_=s_t[:, 2:3]), reads=[s_r], writes=[s_r])
                    fw.op("dve", lambda: nc.vector.tensor_scalar(out=aff_own[:, tt, :], in0=e_t[:], scalar1=s_t[:, 3:4], scalar2=None, op0=ALU.mult), reads=[e_r, s_r], writes=[r_aff])
                fw.dma("sp", aff_in.rearrange("(t p) e -> p t e", p=128), aff_own[:], reads=[r_aff], writes=[rs("aff_in")])
                allgather(aff_in, aff_all, G4, rs("aff_in"), rs("aff_all"))
            fw.barrier()

        if "E" in PHASES:
            with contextlib.ExitStack() as st:
                xeT = sb(st, "xeT", [128, KC, 512], BF16); r_xeT = Res()
                gateS = sb(st, "gateS", [128, 4, 4], F32); r_gate = Res()
                rk = sb(st, "rk", [128, 4, 32], F32); r_rk = Res()
                idx_i = sb(st, "idx_i", [128, 4, 4], I32); r_idx = Res()
                with contextlib.ExitStack() as st1:
                    A_sb = sb(st1, "A_sb", [128, 32, 16], F32); r_A = Res()
                    fw.dma("sp", A_sb[:], aff_all.rearrange("(g p) e -> p g e", p=128), reads=[rs("aff_all")], writes=[r_A])
                    ohEs = sb(st1, "ohEs", [128, 64], F32); r_oh = Res()
                    fw.dma("sp", ohEs[:], ohE[0, :].partition_broadcast(128), writes=[r_oh])
                    lm = sb(st1, "lm", [128, 128], F32); iot = sb(st1, "iot", [128, 512], F32); tok = sb(st1, "tok", [128, 32], F32)
                    fw.dma("sp", lm[:], lowmask, writes=[r_oh]); fw.dma("sp", iot[:], iota512, writes=[r_oh]); fw.dma("sp", tok[:], tokid, writes=[r_oh])
                    vcs = sb(st1, "vcs", [128, 4, 32], F32); r_vcs = Res()
                    fw.op("dve", lambda: nc.vector.memset(vcs[:], 0.0), writes=[r_vcs])
                    for el in range(4):
                        for e in range(16):
                            fw.op("dve", lambda: nc.vector.scalar_tensor_tensor(out=vcs[:, el, :], in0=A_sb[:, :, e], scalar=ohEs[:, el * 16 + e:el * 16 + e + 1], in1=vcs[:, el, :], op0=ALU.mult, op1=ALU.add),
                                  reads=[r_A, r_oh, r_vcs], writes=[r_vcs])
                    bkV = ps(st1, "e_bkV", [128, 512])
                    vt_sb = sb(st1, "vt_sb", [32, 4, 128], F32); r_vt = Res()
                    for el in range(4):
                        fw.op("pe", lambda: nc.tensor.transpose(out=bkV[0:32, el * 128:(el + 1) * 128], in_=vcs[:, el, :], identity=ident_f[:]), reads=[r_vcs, r_ident], writes=[r_vt])
                    fw.op("act", lambda: nc.scalar.copy(out=vt_sb[:], in_=bkV[0:32, 0:512].rearrange("p (e t) -> p e t", e=4)), reads=[r_vt], writes=[r_vt])
                    for el in range(4):
                        fw.dma("sp", vsel[el].rearrange("(g t) -> g t", t=128), vt_sb[:, el, :], reads=[r_vt], writes=[rs("vsel")])
                    vrr = Rot([sb(st1, "vrow%d" % i, [128, 4096], F32) for i in range(2)])
                    junk = sb(st1, "e_junk", [128, 4096], BF16); r_junk = Res()
                    r4 = sb(st1, "r4", [128, 32, 4], F32); r_r4 = Res()
                    TG = sb(st1, "TG", [128, 32, 2], F32); r_TG = Res()
                    OHr = Rot([sb(st1, "OH%d" % i, [128, 128], F32) for i in range(4)])
                    idxg = sb(st1, "idxg", [128, 4, 2], F32); r_ig = Res()
                    bkI = ps(st1, "e_bkI", [128, 512])
                    for el in range(4):
                        vr_, vr_r = vrr.next()
                        fw.dma("sp", vr_[:], vsel[el, :].partition_broadcast(128), reads=[rs("vsel")], writes=[vr_r])
                        fw.op("pool", lambda: nc.gpsimd.memset(r4[:], 0.0), writes=[r_r4])
                        for g in range(32):
                            vc = vcs[:, el, g:g + 1]
                            if g > 0:
                                fw.op("dve", lambda: nc.vector.tensor_scalar(out=junk[:, 0:g * 128], in0=vr_[:, 0:g * 128], scalar1=vc, scalar2=0.0, op0=ALU.is_ge, op1=ALU.add, accum_out=r4[:, g, 0:1]),
                                      reads=[vr_r, r_vcs], writes=[r_junk, r_r4])
                            if g < 31:
                                fw.op("dve", lambda: nc.vector.tensor_scalar(out=junk[:, (g + 1) * 128:4096], in0=vr_[:, (g + 1) * 128:4096], scalar1=vc, scalar2=0.0, op0=ALU.is_gt, op1=ALU.add, accum_out=r4[:, g, 1:2]),
                                      reads=[vr_r, r_vcs], writes=[r_junk, r_r4])
                            fw.op("dve", lambda: nc.vector.tensor_scalar(out=junk[:, g * 128:(g + 1) * 128], in0=vr_[:, g * 128:(g + 1) * 128], scalar1=vc, scalar2=0.0, op0=ALU.is_gt, op1=ALU.add, accum_out=r4[:, g, 2:3]),
                                  reads=[vr_r, r_vcs], writes=[r_junk, r_r4])
                            fw.op("dve", lambda: nc.vector.scalar_tensor_tensor(out=junk[:, g * 128:(g + 1) * 128], in0=vr_[:, g * 128:(g + 1) * 128], scalar=vc, in1=lm[:], op0=ALU.is_equal, op1=ALU.mult, accum_out=r4[:, g, 3:4]),
                                  reads=[vr_r, r_vcs, r_oh], writes=[r_junk, r_r4])
                        fw.op("dve", lambda: nc.vector.tensor_reduce(out=rk[:, el, :], in_=r4[:], axis=AX.X, op=ALU.add), reads=[r_r4], writes=[r_rk])
                        fw.op("pool", lambda: nc.gpsimd.tensor_copy(out=TG[:, :, 0], in_=tok[:, :]), reads=[r_oh], writes=[r_TG])
                        fw.op("pool", lambda: nc.gpsimd.tensor_copy(out=TG[:, :, 1], in_=vcs[:, el, :]), reads=[r_vcs], writes=[r_TG])
                        for s4 in range(4):
                            for g in range(32):
                                OH, OH_r = OHr.next()
                                fw.op("dve", lambda: nc.vector.tensor_scalar(out=OH[:], in0=iot[:, s4 * 128:(s4 + 1) * 128], scalar1=rk[:, el, g:g + 1], scalar2=None, op0=ALU.is_equal), reads=[r_rk, r_oh], writes=[OH_r])
                                fw.op("pe", lambda: nc.tensor.matmul(bkI[:, s4 * 2:(s4 + 1) * 2], lhsT=OH[:], rhs=TG[:, g, :], start=(g == 0), stop=(g == 31)),
                                      reads=[OH_r, r_TG], writes=[r_ig], signal=True)
                        fw.op("act", lambda: nc.scalar.copy(out=idxg[:], in_=bkI[:, 0:8].rearrange("p (s c) -> p s c", c=2)), reads=[r_ig], writes=[r_ig])
                        fw.op("dve", lambda: nc.vector.tensor_scalar(out=idxg[:, :, 0], in0=idxg[:, :, 0], scalar1=0.0, scalar2=4095.0, op0=ALU.max, op1=ALU.min), reads=[r_ig], writes=[r_ig])
                        fw.op("dve", lambda: nc.vector.tensor_copy(out=idx_i[:, el, :], in_=idxg[:, :, 0]), reads=[r_ig], writes=[r_idx])
                        fw.op("dve", lambda: nc.vector.tensor_copy(out=gateS[:, el, :], in_=idxg[:, :, 1]), reads=[r_ig], writes=[r_gate])
                    fw.dma("sp", rank_in, rk[:].rearrange("p a g -> p (a g)"), reads=[r_rk], writes=[rs("rank_in")])
                    allgather(rank_in, rank_all, G4, rs("rank_in"), rs("rank_all"))
                fw.barrier()
                for el in range(4):
                    with contextlib.ExitStack() as st1:
                        xer = Rot([sb(st1, "xe%d" % i, [128, D], BF16) for i in range(2)])
                        ptr = Rot([ps(st1, "e_ptr%d" % i, [128, 1024], BF16) for i in range(2)])
                        for b in range(1):
                            for s4 in range(4):
                                xe, xe_r = xer.next()
                                fw.custom("pool", lambda: nc.gpsimd.indirect_dma_start(
                                    out=xe[:], out_offset=None, in_=h2_all[:, :],
                                    in_offset=bass.IndirectOffsetOnAxis(ap=idx_i[:, el, s4:s4 + 1], axis=0)), 16,
                                    reads=[r_idx, rs("h2_all")], writes=[xe_r])
                                for g8 in range(4):
                                    p_t, p_r = ptr.next()
                                    for k in range(8):
                                        c = g8 * 8 + k
                                        fw.op("pe", lambda: nc.tensor.transpose(out=p_t[:, k * 128:(k + 1) * 128], in_=xe[:, c * 128:(c + 1) * 128], identity=ident_bf[:]),
                                              reads=[xe_r, r_ident], writes=[p_r], signal=(k == 7))
                                    dst = xeT[:, g8 * 8:(g8 + 1) * 8, b * 512 + s4 * 128:b * 512 + (s4 + 1) * 128]
                                    src = p_t[:].rearrange("p (k t) -> p k t", k=8)
                                    if g8 % 2 == 0:
                                        fw.op("act", lambda: nc.scalar.copy(out=dst, in_=src), reads=[p_r], writes=[r_xeT])
                                    else:
                                        fw.op("dve", lambda: nc.vector.tensor_copy(out=dst, in_=src), reads=[p_r], writes=[r_xeT])
                    fw.barrier()
                    with contextlib.ExitStack() as st2:
                        hT_ = sb(st2, "e_hT", [128, 16, 512], BF16); r_hT_ = Res()
                        pp = Rot([ps(st2, "e_ps%d" % i, [128, 512]) for i in range(4)])
                        with contextlib.ExitStack() as st3:
                            WB = 256
                            wg = [sb(st3, "e_wg%d" % i, [128, KC, WB], BF16) for i in range(2)]; r_wg = [Res(), Res()]
                            wu = [sb(st3, "e_wu%d" % i, [128, KC, WB], BF16) for i in range(2)]; r_wu = [Res(), Res()]
                            sar = Rot([sb(st3, "e_sa%d" % i, [128, 512], F32) for i in range(2)])
                            wg_v = w_gate_s[el].rearrange("(c p) f -> p c f", p=128); wu_v = w_up_s[el].rearrange("(c p) f -> p c f", p=128)
                            nb = 2048 // WB
                            fw.dma("pool", wg[0][:], wg_v[:, :, 0:WB], writes=[r_wg[0]]); fw.dma("pool", wu[0][:], wu_v[:, :, 0:WB], writes=[r_wu[0]])
                            for bb in range(nb):
                                if bb + 1 < nb:
                                    s_ = (bb + 1) % 2
                                    fw.dma("pool", wg[s_][:], wg_v[:, :, (bb + 1) * WB:(bb + 2) * WB], writes=[r_wg[s_]])
                                    fw.dma("pool", wu[s_][:], wu_v[:, :, (bb + 1) * WB:(bb + 2) * WB], writes=[r_wu[s_]])
                                for sub in range(WB // 128):
                                    ft = bb * (WB // 128) + sub
                                    for t0 in (0,):
                                        pa, pa_r = pp.next()
                                        mm_fm(pa, pa_r, wg[bb % 2], r_wg[bb % 2], sub * 128, xeT, r_xeT, t0, 512)
                                        pu, pu_r = pp.next()
                                        mm_fm(pu, pu_r, wu[bb % 2], r_wu[bb % 2], sub * 128, xeT, r_xeT, t0, 512)
                                        sa, sa_r = sar.next()
                                        fw.op("act", lambda: nc.scalar.activation(out=sa[:], in_=pa[:], func=AF.Silu), reads=[pa_r], writes=[sa_r])
                                        fw.op("dve", lambda: nc.vector.tensor_tensor(out=hT_[:, ft, t0:t0 + 512], in0=pu[:], in1=sa[:], op=ALU.mult), reads=[pu_r, sa_r], writes=[r_hT_])
                        fw.barrier()
                        with contextlib.ExitStack() as st3:
                            wd = [sb(st3, "e_wd%d" % i, [128, 16, 512], BF16) for i in range(2)]; r_wd = [Res(), Res()]
                            yor = Rot([sb(st3, "e_yo%d" % i, [128, 512], BF16) for i in range(3)])
                            wd_v = w_down_s[el].rearrange("(c p) n -> p c n", p=128)

                            def dn_compute(cb, w_t, w_r):
                                for s8 in range(4):
                                    p_t, p_r = pp.next()
                                    mm_tm(p_t, p_r, w_t, w_r, 0, 512, hT_, r_hT_, s8 * 128, kc=16)
                                    yo, yo_r = yor.next()
                                    fw.op("dve", lambda: nc.vector.tensor_scalar(out=yo[:], in0=p_t[:], scalar1=gateS[:, el, s8:s8 + 1], scalar2=None, op0=ALU.mult), reads=[p_r, r_gate], writes=[yo_r])
                                    r0 = el * 512 + s8 * 128
                                    fw.dma("sp", ye_in[r0:r0 + 128, cb * 512:(cb + 1) * 512], yo[:], reads=[yo_r], writes=[rs("ye_in")])

                            stream_weights(lambda cb: wd_v[:, :, cb * 512:(cb + 1) * 512], 8, wd, r_wd, dn_compute)
                    fw.barrier()
                allgather_chunks(ye_in, ye_all, G4, 16, rs("ye_in"), rs("ye_all"))
            fw.barrier()

        if "C" in PHASES:
            with contextlib.ExitStack() as st:
                Rall = sb(st, "Rall", [128, 4, 128], F32); r_Rall = Res()
                fw.dma("sp", Rall[:], rank_all.rearrange("(c p) f -> p c f", p=128), reads=[rs("rank_all")], writes=[r_Rall])
                ohIs = sb(st, "ohIs", [128, 4], F32); ebs = sb(st, "ebs", [128, 16], F32); r_c0 = Res()
                fw.dma("sp", ohIs[:], ohI[0, :].partition_broadcast(128), writes=[r_c0]); fw.dma("sp", ebs[:], ebase, writes=[r_c0])
                mine = sb(st, "mine", [128, 16, 8], F32); r_mine = Res()
                fw.op("dve", lambda: nc.vector.memset(mine[:], 0.0), writes=[r_mine])
                Rv = Rall[:].rearrange("p c (a g) -> p c a g", a=4)
                for i in range(4):
                    for c in range(4):
                        fw.op("dve", lambda: nc.vector.scalar_tensor_tensor(out=mine[:, 4 * c:4 * c + 4, :], in0=Rv[:, c, :, i * 8:(i + 1) * 8], scalar=ohIs[:, i:i + 1], in1=mine[:, 4 * c:4 * c + 4, :], op0=ALU.mult, op1=ALU.add),
                              reads=[r_Rall, r_c0, r_mine], writes=[r_mine])
                selm = sb(st, "selm", [128, 16, 8], F32); idf = sb(st, "idf", [128, 16, 8], F32); idi = sb(st, "idi", [128, 16, 8], I32)
                fw.op("dve", lambda: nc.vector.tensor_scalar(out=selm[:], in0=mine[:], scalar1=512.0, scalar2=None, op0=ALU.is_lt), reads=[r_mine], writes=[r_mine])
                fw.op("dve", lambda: nc.vector.tensor_scalar(out=idf[:], in0=mine[:], scalar1=511.0, scalar2=0.0, op0=ALU.min, op1=ALU.max), reads=[r_mine], writes=[r_mine])
                qf = sb(st, "qf", [128, 16, 8], F32)
                fw.op("dve", lambda: nc.vector.memset(qf[:], 0.0), writes=[r_mine])
                for kq in range(1, 4):
                    fw.op("dve", lambda: nc.vector.scalar_tensor_tensor(out=qf[:].rearrange("p e g -> p (e g)"), in0=idf[:].rearrange("p e g -> p (e g)"), scalar=128.0 * kq, in1=qf[:].rearrange("p e g -> p (e g)"), op0=ALU.is_ge, op1=ALU.add), reads=[r_mine], writes=[r_mine])
                fw.op("dve", lambda: nc.vector.scalar_tensor_tensor(out=idf[:].rearrange("p e g -> p (e g)"), in0=qf[:].rearrange("p e g -> p (e g)"), scalar=384.0, in1=idf[:].rearrange("p e g -> p (e g)"), op0=ALU.mult, op1=ALU.add), reads=[r_mine], writes=[r_mine])
                for e in range(16):
                    fw.op("dve", lambda: nc.vector.tensor_scalar(out=idf[:, e, :], in0=idf[:, e, :], scalar1=ebs[:, e:e + 1], scalar2=None, op0=ALU.add), reads=[r_mine, r_c0], writes=[r_mine])
                fw.op("dve", lambda: nc.vector.tensor_copy(out=idi[:], in_=idf[:]), reads=[r_mine], writes=[r_mine])
                g2bc = sb(st, "g2bc", [128, D], F32); fnbc = sb(st, "fnbc", [128, D], F32); r_g2 = Res()
                mod_bc(g2bc, 5, 0, r_g2)
                fw.dma("sp", fnbc[:], fnorm_row[0, :].partition_broadcast(128), writes=[r_g2])
                accr = Rot([sb(st, "c_acc%d" % i, [128, D], F32) for i in range(2)])
                xmr = Rot([sb(st, "c_xm%d" % i, [128, D], F32) for i in range(2)])
                gar = Rot([sb(st, "c_ga%d" % i, [128, D], BF16) for i in range(3)])
                cjunk = sb(st, "c_junk", [128, D], BF16); r_cj = Res()
                cst = Rot([sb(st, "c_st%d" % i, [128, 4], F32) for i in range(2)])
                for g in range(8):
                    acc, acc_r = accr.next(); xm, xm_r = xmr.next()
                    fw.dma("sp", xm[:], Xmid[g * 128:(g + 1) * 128, :], reads=[rs("Xmid")], writes=[xm_r])
                    for e in range(16):
                        ga, ga_r = gar.next()
                        fw.custom("pool", lambda: nc.gpsimd.indirect_dma_start(
                            out=ga[:], out_offset=None, in_=ye_all[:, :],
                            in_offset=bass.IndirectOffsetOnAxis(ap=idi[:, e, g:g + 1], axis=0)), 16,
                            reads=[r_mine, rs("ye_all")], writes=[ga_r])
                        if e == 0:
                            fw.op("dve", lambda: nc.vector.tensor_scalar(out=acc[:], in0=ga[:], scalar1=selm[:, e, g:g + 1], scalar2=None, op0=ALU.mult), reads=[ga_r, r_mine], writes=[acc_r])
                        else:
                            fw.op("dve", lambda: nc.vector.scalar_tensor_tensor(out=acc[:], in0=ga[:], scalar=selm[:, e, g:g + 1], in1=acc[:], op0=ALU.mult, op1=ALU.add), reads=[ga_r, r_mine, acc_r], writes=[acc_r])
                    fw.op("dve", lambda: nc.vector.tensor_tensor(out=acc[:], in0=acc[:], in1=g2bc[:], op=ALU.mult), reads=[acc_r, r_g2], writes=[acc_r])
                    fw.op("pool", lambda: nc.gpsimd.tensor_tensor(out=acc[:], in0=acc[:], in1=xm[:], op=ALU.add), reads=[acc_r, xm_r], writes=[acc_r])
                    s_t, s_r = cst.next()
                    fw.op("act", lambda: nc.scalar.activation(out=cjunk[:], in_=acc[:], func=AF.Square, accum_out=s_t[:, 0:1]), reads=[acc_r], writes=[r_cj, s_r])
                    fw.op("act", lambda: nc.scalar.activation(out=s_t[:, 1:2], in_=s_t[:, 0:1], func=AF.Ln, scale=1.0 / D, bias=EPS), reads=[s_r], writes=[s_r])
                    fw.op("act", lambda: nc.scalar.activation(out=s_t[:, 2:3], in_=s_t[:, 1:2], func=AF.Exp, scale=-0.5), reads=[s_r], writes=[s_r])
                    fw.op("dve", lambda: nc.vector.scalar_tensor_tensor(out=xm[:], in0=acc[:], scalar=s_t[:, 2:3], in1=fnbc[:], op0=ALU.mult, op1=ALU.mult), reads=[acc_r, s_r, r_g2, xm_r], writes=[xm_r])
                    fw.dma("sp", out[g * 128:(g + 1) * 128, :], xm[:], reads=[xm_r], writes=[rs("out")])
            fw.barrier()
        fw.barrier()
    return nc


def _host_inputs(inp):
    x = np.asarray(inp["x"], np.float32); c = np.asarray(inp["c"], np.float32)
    ctx = np.asarray(inp["ctx"], np.float32); c_ctx = np.asarray(inp["c_ctx"], np.float32)
    w_mod = np.asarray(inp["w_mod"], np.float32)[0]; b_mod = np.asarray(inp["b_mod"], np.float32)[0]
    w_in = np.ascontiguousarray(np.asarray(inp["w_in"], np.float32)[0])
    rpb = np.asarray(inp["na_rpb"], np.float32)[0]
    common = {}
    common["w_in"] = w_in
    common["norm1_row"] = np.asarray(inp["norm1"], np.float32).reshape(1, D)
    common["norm2_row"] = np.asarray(inp["norm2"], np.float32).reshape(1, D)
    common["fnorm_row"] = np.asarray(inp["final_norm"], np.float32).reshape(1, D)
    common["ret_decay"] = np.asarray(inp["ret_decay"], np.float32).reshape(1, 16)
    common["w_bna"] = np.ascontiguousarray(np.asarray(inp["w_branch_na"], np.float32)[0])
    common["w_bret"] = np.ascontiguousarray(np.asarray(inp["w_branch_ret"], np.float32)[0])
    common["w_out"] = np.ascontiguousarray(np.asarray(inp["w_out"], np.float32)[0])
    common["w_router"] = np.ascontiguousarray(np.asarray(inp["w_router"], np.float32)[0])
    common["ident_in"] = np.eye(128, dtype=np.float32)
    common["iota512"] = np.tile(np.arange(512, dtype=np.float32)[None], (128, 1))
    pp_ = np.arange(128)
    common["lowmask"] = (pp_[None, :] < pp_[:, None]).astype(np.float32)
    kc = np.arange(64); qc = np.arange(64)
    c0 = np.clip(qc - 8, 0, 48)
    col_ok = (kc[:, None] >= c0[None, :]) & (kc[:, None] < c0[None, :] + 16)
    dc = np.clip(kc[:, None] - qc[None, :], -15, 15) + 15
    nab = np.full((16, 2, 64, 7, 2, 64), -1e30, np.float32)
    for di in range(7):
        delta = 2 * di - 2
        for sr in range(2):
            for rr in range(2):
                dr = delta + sr - rr + 3
                if 0 <= dr <= 14:
                    blk = rpb[:, dr][:, dc]
                    nab[:, sr, :, di, rr, :] = np.where(col_ok[None], blk, np.float32(-1e30))
    common["nabias"] = nab.reshape(16, 128, 7 * 128)
    oh = np.zeros((128, 24, 64), np.float32)
    for s in range(24):
        oh[s, s, :] = 1.0
    common["onehotA"] = oh.reshape(128, NKV)
    p = np.arange(128, dtype=np.float32)
    Mf = np.where(p[None, :] >= p[:, None], p[None, :] - p[:, None], 1e9).astype(np.float32)
    Mb = np.where(p[:, None] >= p[None, :], p[:, None] - p[None, :], 1e9).astype(np.float32)
    common["retM"] = np.concatenate([Mf, Mb], axis=1)
    rp = np.zeros((128, 26), np.float32)
    rp[:, 0] = p + 1; rp[:, 1] = 127 - p; rp[:, 2] = 128.0
    rp[:, 13] = 128 - p; rp[:, 14] = p; rp[:, 15] = 128.0
    for t in range(8):
        rp[:, 3 + t] = 1023 - (t * 128 + p)
        rp[:, 16 + t] = t * 128 + p
    rp[:, 11] = 255 - p; rp[:, 12] = 127 - p
    rp[:, 24] = p; rp[:, 25] = 128 + p
    common["retpos"] = rp
    inv = (10000.0 ** (-np.arange(64, dtype=np.float32) / 64)).astype(np.float32)
    per_core = []
    wg = np.asarray(inp["w_gate"], np.float32)[0]; wu = np.asarray(inp["w_up"], np.float32)[0]; wd = np.asarray(inp["w_down"], np.float32)[0]
    for j in range(8):
        b = j // 4; jj = j % 4
        d = dict(common)
        d["x_own"] = np.ascontiguousarray(x[b, jj * 1024:(jj + 1) * 1024])
        xkv = np.zeros((24, 64, D), np.float32)
        for s in range(24):
            KR = 16 * jj - 4 + s
            if 0 <= KR < 64:
                xkv[s] = x[b, KR * 64:(KR + 1) * 64]
        d["x_kv"] = xkv.reshape(NKV, D)
        d["ctxb"] = np.ascontiguousarray(ctx[b])
        cc = np.stack([c[b], c_ctx], axis=-1)
        d["cT"] = np.ascontiguousarray(cc.reshape(KC, 128, 2).transpose(1, 0, 2))
        d["w_mod_s"] = np.ascontiguousarray(w_mod[:, jj * 6144:(jj + 1) * 6144])
        d["bmod_row"] = np.ascontiguousarray(b_mod[jj * 6144:(jj + 1) * 6144]).reshape(1, 6144)
        t = np.arange(NTOK)
        Rw = (16 * jj + t // 64).astype(np.float32); Cl = (t % 64).astype(np.float32)
        ang_r = Rw[:, None] * inv[None, :]; ang_c = Cl[:, None] * inv[None, :]
        cs = np.concatenate([np.cos(ang_r), np.cos(ang_c)], axis=1).astype(np.float32)
        sn = np.concatenate([np.sin(ang_r), np.sin(ang_c)], axis=1).astype(np.float32)
        d["ropeC"] = cs; d["ropeS"] = sn
        d["ropeCk"] = (cs / 16.0).astype(np.float32); d["ropeSk"] = (sn / 16.0).astype(np.float32)
        vb = np.zeros((128, 16, 64), np.float32)
        for r in range(16):
            Rg = 16 * jj + r
            r0 = min(max(Rg - 4, 0), 56)
            for s in range(24):
                KR = 16 * jj - 4 + s
                if not (r0 <= KR < r0 + 8):
                    vb[s, r, :] = -30000.0
        d["validB"] = vb.reshape(128, NTOK)
        E = np.zeros((1, 10), np.float32); M = np.zeros((1, 10), np.float32)
        for i in range(4):
            if i < jj:
                E[0, i] = 1024.0 * (jj - 1 - i); M[0, i] = 1.0
            if i > jj:
                E[0, 5 + i] = 1024.0 * (i - jj - 1); M[0, 5 + i] = 1.0
        E[0, 4] = 1024.0 * jj; M[0, 4] = 1.0
        E[0, 9] = 1024.0 * (3 - jj); M[0, 9] = 1.0
        d["ccE"] = E; d["ccM"] = M
        d["w_gate_s"] = np.ascontiguousarray(wg[4 * jj:4 * jj + 4]); d["w_up_s"] = np.ascontiguousarray(wu[4 * jj:4 * jj + 4])
        d["w_down_s"] = np.ascontiguousarray(wd[4 * jj:4 * jj + 4])
        oe = np.zeros((1, 64), np.float32)
        for el in range(4):
            oe[0, el * 16 + 4 * jj + el] = 1.0
        d["ohE"] = oe
        oi = np.zeros((1, 4), np.float32); oi[0, jj] = 1.0
        d["ohI"] = oi
        g = np.arange(32)
        ntok_ = g[None, :] * 128 + np.arange(128)[:, None]
        ci = ntok_ // 1024; tl = ntok_ % 1024
        d["tokid"] = ((tl // 128) * 512 + ci * 128 + (tl % 128)).astype(np.float32)
        eb = np.zeros((128, 16), np.float32)
        for e in range(16):
            eb[:, e] = ((e % 4) * 4) * 512 + (e // 4) * 128
        d["ebase"] = eb
        per_core.append(d)
    return per_core


_NC = None


def kernel(**inputs):
    global _NC
    per_core = _host_inputs(inputs)
    if _NC is None:
        _NC = build_nc()
    res = run_bass_kernel_spmd(_NC, per_core, core_ids=list(range(8)))
    outs = [np.asarray(r["out"], np.float32) for r in res.results]
    full = np.zeros((2, 4096, D), np.float32)
    for j in range(8):
        full[j // 4, (j % 4) * 1024:(j % 4 + 1) * 1024] = outs[j]
    return full
```
